# Optimizing a Trainium2 kernel written in Bass

```python
import math
import jax, jax.numpy as jnp
from jax import lax
import numpy as np

D_MODEL = 1024
BATCH = 8
SEQ = 8192
DEPTH = 1

ATTN_HEADS = 8
ATTN_KV_HEADS = 2
ATTN_HEAD_DIM = 64
Q_PER_KV = ATTN_HEADS // ATTN_KV_HEADS
WINDOW = 128
ATTN_BLOCK = WINDOW
ATTN_WIDTH = ATTN_HEADS * ATTN_HEAD_DIM
KV_WIDTH = ATTN_KV_HEADS * ATTN_HEAD_DIM
HGRN_HEADS = 4
HGRN_KEY_DIM = 128
HGRN_VAL_DIM = 128
HGRN_WIDTH = HGRN_HEADS * HGRN_VAL_DIM
HGRN_KEY_WIDTH = HGRN_HEADS * HGRN_KEY_DIM
HGRN_CHUNK = 32
MIX_WIDTH = ATTN_WIDTH + HGRN_WIDTH
IN_SPLITS = [ATTN_WIDTH, KV_WIDTH, KV_WIDTH, HGRN_KEY_WIDTH, HGRN_KEY_WIDTH, HGRN_WIDTH, HGRN_WIDTH]
IN_WIDTH = sum(IN_SPLITS)
N_GROUPS = 4
EXPERTS_PER_GROUP = 8
N_EXPERTS = N_GROUPS * EXPERTS_PER_GROUP
TOP_K = 2
EXPERT_FF = D_MODEL // 4

NORM_EPS = 1e-6
NEG_INF = -1e30

kernel_name = "hymba_swa_sink_hgrn2_hmoe_layer"


def rmsnorm(x, g):
    xf = x.astype(jnp.float32)
    y = xf * lax.rsqrt(jnp.mean(xf * xf, axis=-1, keepdims=True) + NORM_EPS)
    return (y * g.astype(jnp.float32)).astype(x.dtype)


def swa_sink_alibi(q, k, v, sinks):
    B, T, _ = q.shape
    NB = T // ATTN_BLOCK
    qb = q.reshape(B, NB, ATTN_BLOCK, ATTN_KV_HEADS, Q_PER_KV, ATTN_HEAD_DIM)
    pad = ((0, 0), (ATTN_BLOCK, 0), (0, 0))
    kp = jnp.pad(k, pad).reshape(B, NB + 1, ATTN_BLOCK, ATTN_KV_HEADS, ATTN_HEAD_DIM)
    vp = jnp.pad(v, pad).reshape(B, NB + 1, ATTN_BLOCK, ATTN_KV_HEADS, ATTN_HEAD_DIM)
    kb = jnp.concatenate([kp[:, :-1], kp[:, 1:]], axis=2)
    vb = jnp.concatenate([vp[:, :-1], vp[:, 1:]], axis=2)
    scale = 1.0 / math.sqrt(ATTN_HEAD_DIM)
    s = jnp.einsum('bnqgrd,bnkgd->bngrqk', qb, kb, preferred_element_type=jnp.float32) * scale
    qi = jnp.arange(ATTN_BLOCK)[:, None]
    kj = jnp.arange(2 * ATTN_BLOCK)[None, :]
    dist = qi + ATTN_BLOCK - kj
    in_win = (dist >= 0) & (dist < WINDOW)
    not_pad = (jnp.arange(NB)[:, None, None] > 0) | (kj >= ATTN_BLOCK)[None]
    valid = in_win[None] & not_pad
    slopes = jnp.exp2(-8.0 * (jnp.arange(ATTN_HEADS, dtype=jnp.float32) + 1.0) / ATTN_HEADS)
    slopes = slopes.reshape(ATTN_KV_HEADS, Q_PER_KV)[:, :, None, None]
    s = s - slopes * dist.astype(jnp.float32)
    s = jnp.where(valid[None, :, None, None], s, NEG_INF)
    sink = sinks.astype(jnp.float32).reshape(ATTN_KV_HEADS, Q_PER_KV)[:, :, None, None]
    m = jnp.maximum(jnp.max(s, axis=-1, keepdims=True), sink)
    p = jnp.exp(s - m)
    p = p / (jnp.sum(p, axis=-1, keepdims=True) + jnp.exp(sink - m))
    o = jnp.einsum('bngrqk,bnkgd->bnqgrd', p.astype(v.dtype), vb)
    return o.reshape(B, T, ATTN_WIDTH)


def hgrn2(q_raw, f_raw, i_raw, lb):
    B, T, _ = q_raw.shape
    NC = T // HGRN_CHUNK
    shp_k = (B, NC, HGRN_CHUNK, HGRN_HEADS, HGRN_KEY_DIM)
    q = jax.nn.silu(q_raw.astype(jnp.float32)).reshape(shp_k)
    lbf = lb.astype(jnp.float32)
    f = lbf + (1.0 - lbf) * jax.nn.sigmoid(f_raw.astype(jnp.float32))
    k = (1.0 - f).reshape(shp_k)
    logf = jnp.log(f).reshape(shp_k)
    v = i_raw.astype(jnp.float32).reshape(B, NC, HGRN_CHUNK, HGRN_HEADS, HGRN_VAL_DIM)
    b = jnp.cumsum(logf, axis=2)
    b_last = b[:, :, -1:]
    q_dec = q * jnp.exp(b)
    k_dec = k * jnp.exp(-b)
    k_end = k * jnp.exp(b_last - b)
    causal = jnp.tril(jnp.ones((HGRN_CHUNK, HGRN_CHUNK), dtype=bool))
    a = jnp.einsum('bnchk,bnshk->bnhcs', q_dec, k_dec)
    a = jnp.where(causal, a, 0.0)
    o_intra = jnp.einsum('bnhcs,bnshv->bnchv', a, v)

    def step(S, inp):
        qd, ke, vv, decay = inp
        o = jnp.einsum('bchk,bhkv->bchv', qd, S)
        S = S * decay[..., None] + jnp.einsum('bchk,bchv->bhkv', ke, vv)
        return S, o

    S0 = jnp.zeros((B, HGRN_HEADS, HGRN_KEY_DIM, HGRN_VAL_DIM), jnp.float32)
    xs = (jnp.moveaxis(q_dec, 1, 0), jnp.moveaxis(k_end, 1, 0), jnp.moveaxis(v, 1, 0),
          jnp.moveaxis(jnp.exp(b_last[:, :, 0]), 1, 0))
    _, o_inter = lax.scan(step, S0, xs)
    o = o_intra + jnp.moveaxis(o_inter, 0, 1)
    return o.reshape(B, T, HGRN_HEADS, HGRN_VAL_DIM).astype(q_raw.dtype)


def hierarchical_moe(h, w_rg, b_rg, w_re, b_re, w_gate, w_up, w_down):
    B, T, D = h.shape
    tok = h.reshape(-1, D)
    N = tok.shape[0]
    g_prob = jax.nn.softmax((tok @ w_rg + b_rg).astype(jnp.float32), axis=-1)
    g_w, g_idx = lax.top_k(g_prob, 1)
    e_logits = (tok @ w_re + b_re).astype(jnp.float32).reshape(N, N_GROUPS, EXPERTS_PER_GROUP)
    e_sel = e_logits[jnp.arange(N), g_idx[:, 0]]
    e_w, e_idx = lax.top_k(jax.nn.softmax(e_sel, axis=-1), TOP_K)
    e_w = e_w / jnp.sum(e_w, axis=-1, keepdims=True)
    weights = (g_w * e_w).astype(h.dtype)
    expert = (g_idx * EXPERTS_PER_GROUP + e_idx).reshape(-1)
    order = jnp.argsort(expert)
    inv = jnp.argsort(order)
    xs = tok[order // TOP_K]
    sizes = jnp.bincount(expert, length=N_EXPERTS).astype(jnp.int32)
    hg = lax.ragged_dot(xs, w_gate, sizes)
    hu = lax.ragged_dot(xs, w_up, sizes)
    ys = lax.ragged_dot(jax.nn.silu(hg) * hu, w_down, sizes)
    ys = ys[inv].reshape(N, TOP_K, D)
    out = jnp.einsum('nkd,nk->nd', ys, weights)
    return out.reshape(B, T, D)


def setup_inputs(seed: int = 0) -> dict:
    key = jax.random.key(seed)
    ks = jax.random.split(key, 24)
    f32 = jnp.float32
    L, D = DEPTH, D_MODEL

    def nrm(k, shape, scale):
        return jax.random.normal(k, shape, f32) * scale

    return {
        "x": nrm(ks[0], (BATCH, SEQ, D), 1.0),
        "c": nrm(ks[1], (BATCH, D), 1.0),
        "ln1_pre": 1.0 + nrm(ks[2], (L, D), 0.02),
        "ln1_post": 1.0 + nrm(ks[3], (L, D), 0.02),
        "ln2_pre": 1.0 + nrm(ks[4], (L, D), 0.02),
        "ln2_post": 1.0 + nrm(ks[5], (L, D), 0.02),
        "w_ada": nrm(ks[6], (L, D, 6 * D), 0.25 * D ** -0.5),
        "b_ada": nrm(ks[7], (L, 6 * D), 0.01),
        "w_in": nrm(ks[8], (L, D, IN_WIDTH), D ** -0.5),
        "attn_sinks": nrm(ks[9], (L, ATTN_HEADS), 0.5),
        "attn_out_norm": 1.0 + nrm(ks[10], (L, ATTN_WIDTH), 0.02),
        "hgrn_lb": nrm(ks[11], (L + 1, HGRN_KEY_WIDTH), 0.1),
        "hgrn_out_norm": 1.0 + nrm(ks[12], (L, HGRN_WIDTH), 0.02),
        "w_out": nrm(ks[13], (L, MIX_WIDTH, D), MIX_WIDTH ** -0.5),
        "w_router_group": nrm(ks[14], (L, D, N_GROUPS), D ** -0.5),
        "b_router_group": nrm(ks[15], (L, N_GROUPS), 0.01),
        "w_router_expert": nrm(ks[16], (L, D, N_EXPERTS), D ** -0.5),
        "b_router_expert": nrm(ks[17], (L, N_EXPERTS), 0.01),
        "w_exp_gate": nrm(ks[18], (L, N_EXPERTS, D, EXPERT_FF), D ** -0.5),
        "w_exp_up": nrm(ks[19], (L, N_EXPERTS, D, EXPERT_FF), D ** -0.5),
        "w_exp_down": nrm(ks[20], (L, N_EXPERTS, EXPERT_FF, D), EXPERT_FF ** -0.5),
    }


def reference(x, c, ln1_pre, ln1_post, ln2_pre, ln2_post, w_ada, b_ada, w_in, attn_sinks,
              attn_out_norm, hgrn_lb, hgrn_out_norm, w_out, w_router_group, b_router_group,
              w_router_expert, b_router_expert, w_exp_gate, w_exp_up, w_exp_down):
    B, T, D = x.shape
    lb_all = jnp.cumsum(jax.nn.softmax(hgrn_lb.astype(jnp.float32), axis=0), axis=0)
    offsets = list(np.cumsum(IN_SPLITS)[:-1])
    c_act = jax.nn.silu(c)
    for l in range(DEPTH):
        mod = (c_act @ w_ada[l] + b_ada[l])[:, None, :]
        sh1, sc1, ga1, sh2, sc2, ga2 = jnp.split(mod, 6, axis=-1)
        h = rmsnorm(x, ln1_pre[l]) * (1.0 + sc1) + sh1
        proj = h @ w_in[l]
        q_a, k_a, v_a, q_h, f_h, i_h, g_h = jnp.split(proj, offsets, axis=-1)
        attn = rmsnorm(swa_sink_alibi(q_a, k_a, v_a, attn_sinks[l]), attn_out_norm[l])
        hg = hgrn2(q_h, f_h, i_h, lb_all[l])
        hg = rmsnorm(hg, hgrn_out_norm[l].reshape(HGRN_HEADS, HGRN_VAL_DIM)).reshape(B, T, HGRN_WIDTH)
        hg = hg * jax.nn.silu(g_h)
        mix = jnp.concatenate([attn, hg], axis=-1) @ w_out[l]
        x = x + ga1 * rmsnorm(mix, ln1_post[l])
        h = rmsnorm(x, ln2_pre[l]) * (1.0 + sc2) + sh2
        y = hierarchical_moe(h, w_router_group[l], b_router_group[l], w_router_expert[l],
                             b_router_expert[l], w_exp_gate[l], w_exp_up[l], w_exp_down[l])
        x = x + ga2 * rmsnorm(y, ln2_post[l])
    return x
```

```python
import os as _os
import numpy as np
import ml_dtypes
from contextlib import ExitStack
import concourse.bass as bass
import concourse.mybir as mybir
from concourse.bass_utils import run_bass_kernel_spmd
from concourse.alu_op_type import AluOpType as ALU

AF = mybir.ActivationFunctionType
AX = mybir.AxisListType
F32 = mybir.dt.float32
BF16 = mybir.dt.bfloat16
I32 = mybir.dt.int32
U32 = mybir.dt.uint32

NDSEM = 40
NWB = int(_os.environ.get('NWB', '3'))
SCHED = _os.environ.get("NOSCHED") != "1"
SCHED_SEGS = _os.environ.get('SCHED_SEGS', 'all')
SBUF_BASE = 16512
SBUF_LIMIT = 229344


def dtsize(dt):
    return 2 if dt == BF16 else 4


class Op:
    __slots__ = ("eng", "fn", "dma", "deps", "alldeps", "inc", "cnt", "dsem", "dval", "dprev", "cost", "dmat", "barwait", "idx", "psrd", "pstag", "t0", "t1", "tset")


PRI_MODE = _os.environ.get('PRI_MODE', 'cp')
PRI_CP_SEGS = _os.environ.get('PRI_CP_SEGS', 'all')
PRI_K = float(_os.environ.get('PRI_K', '0.05'))
TBL_AWARE = _os.environ.get('TBL_AWARE', '0') == '1'
TBL_WINDOW = int(_os.environ.get('TBL_WINDOW', '200'))
SYNC_LAT = float(_os.environ.get('SYNC_LAT', '0.15'))
DMA_LAT = 2.0


PS_CUR = {}
BC_REG = [None]


class Prog:
    def __init__(self):
        self.ops = []
        self.res = {}
        self.segs = [0]
        self.fixed = set()

    def add(self, eng, fn, reads=(), writes=(), dma=False, cost=0.1, dmat=0.0, tset=None):
        op = Op()
        op.tset = tset
        op.eng, op.fn, op.dma, op.inc, op.cnt = eng, fn, dma, False, 0
        op.cost, op.dmat, op.barwait = cost, dmat, False
        op.psrd = any(isinstance(r, str) and r.startswith("ps") for r in reads)
        op.pstag = None
        for r in list(reads) + list(writes):
            if isinstance(r, str) and r.startswith("ps") and r in PS_CUR:
                op.pstag = PS_CUR[r]
        _nw = _os.environ.get("ANALYZE_NOWAR_KEYS")

        def relaxed(key):
            if not _nw:
                return False
            wk = key if isinstance(key, str) else str(key[0])
            if _nw == "ALL":
                return True
            if _nw == "SBUF":
                return not wk.startswith("ps")
            return any(wk.startswith(p) for p in _nw.split(","))
        deps = []
        for r in reads:
            st = self.res.get(r)
            if st is None:
                st = self.res[r] = [None, {}, []]
            if st[0] is not None:
                deps.append(st[0])
            if isinstance(r, str) and r.startswith("ps") and not relaxed(r):
                for lst in st[1].values():
                    deps.extend(lst)
        for w in writes:
            st = self.res.get(w)
            if st is None:
                st = self.res[w] = [None, {}, []]
            if relaxed(w):
                continue
            if st[0] is not None:
                deps.append(st[0])
            for lst in st[1].values():
                deps.extend(lst)
            deps.extend(st[2])
        for r in reads:
            st = self.res[r]
            if dma:
                st[2].append(op)
            else:
                st[1].setdefault(eng, []).append(op)
        for w in writes:
            st = self.res[w]
            st[0] = op
            st[1] = {}
            st[2] = []
        seen = set()
        d2 = []
        dall = []
        for d in deps:
            if id(d) in seen or d is op:
                continue
            seen.add(id(d))
            dall.append(d)
            if (not d.dma) and (not dma) and d.eng == eng and eng == "pe":
                continue
            d2.append(d)
        op.deps = d2
        op.alldeps = dall
        self.ops.append(op)
        return op

    def barrier(self, markers):
        ms = []
        self.segs.append(len(self.ops))
        self.fixed.add(len(self.segs) - 1)
        for n, (eng, fn) in enumerate(markers):
            m = self.add(eng, fn, writes=[("bar", len(self.ops), n)])
            m.inc = True
            ms.append(m)
        for eng in ["pe", "act", "pool", "dve", "sp"]:
            b = self.add(eng, None)
            b.deps = list(ms)
            b.alldeps = list(ms)
            b.barwait = True
        self.segs.append(len(self.ops))

    def schedule(self):
        import heapq
        bounds = self.segs + [len(self.ops)]
        new_ops = []
        for si in range(len(bounds) - 1):
            ops = self.ops[bounds[si]:bounds[si + 1]]
            n = len(ops)
            if n == 0:
                continue
            if si in self.fixed or (SCHED_SEGS != 'all' and str(si) not in SCHED_SEGS.split(',')):
                new_ops.extend(ops)
                continue
            pos = {id(op): k for k, op in enumerate(ops)}
            succ = [[] for _ in range(n)]
            npred = [0] * n
            chain = _os.environ.get('SCHED_CHAIN', '').split(',')
            lastk = {}
            for k, op in enumerate(ops):
                for d in op.alldeps:
                    j = pos.get(id(d))
                    if j is not None:
                        succ[j].append(k)
                        npred[k] += 1
                if op.eng in chain:
                    if op.eng in lastk:
                        succ[lastk[op.eng]].append(k)
                        npred[k] += 1
                    lastk[op.eng] = k
            ready = {e: [] for e in ["pe", "act", "pool", "dve", "sp"]}
            rtime = [0.0] * n
            PSB_ = int(_os.environ.get("PS_BONUS", "0"))
            pri = [k - (PSB_ if ops[k].psrd else 0) for k in range(n)]
            if PRI_MODE == "cp" and (PRI_CP_SEGS == "all" or str(si) in PRI_CP_SEGS.split(",")):
                bl = [0.0] * n
                for k in range(n - 1, -1, -1):
                    m_ = 0.0
                    for q_ in succ[k]:
                        if bl[q_] > m_:
                            m_ = bl[q_]
                    c_ = ops[k].cost + (ops[k].dmat + DMA_LAT if ops[k].dma else 0.0)
                    bl[k] = c_ + SYNC_LAT + m_
                w_ = float(_os.environ.get("PRI_W", "1.0"))
                pri = [k * PRI_K - w_ * bl[k] for k in range(n)]
            for k in range(n):
                if npred[k] == 0:
                    heapq.heappush(ready[ops[k].eng], k)
            eng_free = {e: 0.0 for e in ready}
            cur_tset = [None]
            nloads = 0
            start = [0.0] * n
            events = []
            dma_free = 0.0
            t = 0.0
            done = 0
            while done < n:
                while events and events[0][0] <= t + 1e-9:
                    ft, k = heapq.heappop(events)
                    done += 1
                    for m in succ[k]:
                        npred[m] -= 1
                        rt = ft + (0.0 if (ops[m].eng == ops[k].eng and ops[k].eng == "pe") else SYNC_LAT)
                        if rt > rtime[m]:
                            rtime[m] = rt
                        if npred[m] == 0:
                            heapq.heappush(ready[ops[m].eng], m)
                for e in ready:
                    if eng_free[e] > t + 1e-9 or not ready[e]:
                        continue
                    cand = [k for k in ready[e] if rtime[k] <= t + 1e-9]
                    if not cand:
                        continue
                    if e == "act":
                        oldest = min(cand)
                        same = [c for c in cand if ops[c].tset is None or ops[c].tset == cur_tset[0]]
                        if TBL_AWARE and same and (min(same) - oldest) < TBL_WINDOW:
                            k = min(same)
                        else:
                            k = min(cand, key=lambda c: (pri[c], c))
                        if ops[k].tset is not None and ops[k].tset != cur_tset[0]:
                            cur_tset[0] = ops[k].tset
                            t_extra = 1.3 if TBL_AWARE else 0.0
                            nloads += 1
                        else:
                            t_extra = 0.0
                    else:
                        k = min(cand, key=lambda c: (pri[c], c))
                        t_extra = 0.0
                    ready[e].remove(k)
                    heapq.heapify(ready[e])
                    op = ops[k]
                    start[k] = t
                    op.t0 = t
                    eng_free[e] = t + op.cost + t_extra
                    if op.dma:
                        b = max(dma_free, t + op.cost)
                        dma_free = b + op.dmat
                        fin = dma_free + DMA_LAT
                    else:
                        fin = t + op.cost + t_extra
                    op.t1 = fin
                    heapq.heappush(events, (fin, k))
                nxt = []
                if events:
                    nxt.append(events[0][0])
                for e in ready:
                    if ready[e]:
                        tm = max(eng_free[e], min(rtime[k] for k in ready[e]))
                        nxt.append(tm)
                if not nxt:
                    break
                t2 = min(nxt)
                t = t2 if t2 > t else t + 1e-6
            if _os.environ.get("SIM_DBG") == "1" and si == 0:
                firsts = {}
                for k in range(n):
                    op = ops[k]
                    key = (op.eng, round(op.cost, 3), op.dma)
                    if key not in firsts:
                        firsts[key] = (start[k], k)
                for key, v in sorted(firsts.items(), key=lambda kv: kv[1][0])[:40]:
                    print("     first", key, "t0=%.1f idx=%d" % v)
            order = sorted(range(n), key=lambda k: (start[k], k))
            new_ops.extend(ops[k] for k in order)
            tot = {}
            for op in ops:
                tot[op.eng] = tot.get(op.eng, 0.0) + op.cost
            if _os.environ.get("PS_STATS") == "1":
                iv = {}
                for op in ops:
                    if op.pstag is not None:
                        a_, b_ = iv.get(op.pstag, (1e18, 0.0))
                        iv[op.pstag] = (min(a_, op.t0), max(b_, op.t1))
                agg = {}
                for (ser, tag), (a_, b_) in iv.items():
                    agg.setdefault(tag, []).append(b_ - a_)
                for tag, v in sorted(agg.items(), key=lambda kv: -sum(kv[1])):
                    print("    ps hold %-10s n=%4d mean %.2f us total %.0f" % (tag, len(v), sum(v) / len(v), sum(v)))
            print("  segment %d: %d ops, tblloads %d, simulated makespan %.1f us; busy per engine: %s; dma busy %.1f" % (
                si, n, nloads, max(eng_free.values()), {e: round(v, 1) for e, v in tot.items()}, sum(op.dmat for op in ops if op.dma)))
        self.ops = new_ops

    def emit(self, nc, stack):
        engs = ["pe", "act", "pool", "dve", "sp"]
        csem = {e: stack.enter_context(nc.semaphore("c_" + e)) for e in engs}
        dsem = [stack.enter_context(nc.semaphore("d_%d" % i)) for i in range(NDSEM)]
        for i_, op in enumerate(self.ops):
            op.idx = i_
        for op in self.ops:
            best = {}
            nd = []
            for d in op.deps:
                if d.dma:
                    nd.append(d)
                else:
                    b_ = best.get(d.eng)
                    if b_ is None or d.idx > b_.idx:
                        best[d.eng] = d
            for d in best.values():
                assert d.idx < op.idx
                d.inc = True
                nd.append(d)
            op.deps = nd
        cnt = {e: 0 for e in engs}
        uses = [0] * NDSEM
        ndma = 0
        snap = {}
        for op in self.ops:
            if op.dma:
                slot = ndma % NDSEM
                ndma += 1
                op.dsem = slot
                op.dprev = 16 * uses[slot]
                uses[slot] += 1
                op.dval = 16 * uses[slot]
            elif op.inc:
                cnt[op.eng] += 1
                op.cnt = cnt[op.eng]
            if op.barwait:
                snap[id(op)] = list(uses)
        per = {e: [] for e in engs}
        for op in self.ops:
            per[op.eng].append(op)
        nwait = [0]

        def run(e, name):
            known = {}
            if name == "pool":
                BC_REG[0] = e.alloc_register(name="bcreg")
                e.reg_mov(BC_REG[0], 4095)
            for op in per[name]:
                waits = {}
                for d in op.deps:
                    if d.dma:
                        key, val = ("d", d.dsem), d.dval
                    else:
                        key, val = ("c", d.eng), d.cnt
                    if known.get(key, 0) >= val:
                        continue
                    if waits.get(key, 0) < val:
                        waits[key] = val
                if op.dma and op.dprev > 0:
                    key = ("d", op.dsem)
                    if known.get(key, 0) < op.dprev and waits.get(key, 0) < op.dprev:
                        waits[key] = op.dprev
                if op.barwait:
                    for slot, u in enumerate(snap[id(op)]):
                        key = ("d", slot)
                        if u > 0 and known.get(key, 0) < 16 * u:
                            waits[key] = 16 * u
                for key, val in waits.items():
                    s = dsem[key[1]] if key[0] == "d" else csem[key[1]]
                    e.wait_ge(s, val)
                    known[key] = val
                    nwait[0] += 1
                if op.fn is None:
                    continue
                ins = op.fn(e)
                if op.dma:
                    ins.then_inc(dsem[op.dsem], 16)
                elif op.inc:
                    ins.then_inc(csem[name], 1)

        with nc.Block() as block:
            @block.tensor
            def _(e):
                run(e, "pe")

            @block.scalar
            def _(e):
                run(e, "act")

            @block.gpsimd
            def _(e):
                run(e, "pool")

            @block.vector
            def _(e):
                run(e, "dve")

            @block.sync
            def _(e):
                run(e, "sp")
        return {e: len(per[e]) for e in engs}, nwait[0]


class Arena:
    def __init__(self, nc, base):
        self.nc, self.top = nc, base
        self.offs = {}

    def t(self, name, shape, dt):
        nbytes = int(np.prod(shape[1:])) * dtsize(dt)
        off = (self.top + 31) // 32 * 32
        self.top = off + nbytes
        assert self.top <= SBUF_LIMIT, (name, self.top)
        self.offs[name] = off
        return self.nc.alloc_sbuf_tensor_at(name, list(shape), dt, offset=off)

    def at(self, name, shape, dt, base_name, extra=0):
        return self.nc.alloc_sbuf_tensor_at(name, list(shape), dt, offset=self.offs[base_name] + extra)


def cf_layout(NT):
    NSLT = 2 * NT + 32
    L = {}
    o = 0
    for name, n in [("E", 2048), ("RM", 512), ("MJ", 4), ("IO32", 32), ("PIDX", 1), ("EPS", 1),
                    ("ONE", 1), ("NEG", 4), ("MG", NT), ("JG", NSLT)]:
        L[name] = o
        o += n
    L["N"] = o
    return L


def make_consts(NT):
    NSLT = 2 * NT + 32
    L = cf_layout(NT)
    cf = np.zeros((128, L["N"]), np.float32)
    k = np.arange(128)[:, None]
    q = np.arange(128)[None, :]
    for kb in range(2):
        for g in range(2):
            for r in range(4):
                h = 4 * g + r
                slope = 2.0 ** (-(h + 1))
                if kb == 1:
                    dist = q - k
                    valid = k <= q
                else:
                    dist = q + 128 - k
                    valid = k > q
                e = np.where(valid, np.exp(-slope * dist.astype(np.float64)), 0.0)
                c0 = L["E"] + (kb * 2 + g) * 512 + r * 128
                cf[:, c0:c0 + 128] = e
    rm = np.ones(512, np.float32)
    rm[::32] = 0.0
    cf[:, L["RM"]:L["RM"] + 512] = rm[None, :]
    for j in range(4):
        cf[32 * j:32 * j + 32, L["MJ"] + j] = 1.0
    cf[:, L["IO32"]:L["IO32"] + 32] = np.arange(32)[None, :]
    cf[:, L["PIDX"]] = np.arange(128)
    cf[:, L["EPS"]] = 1e-6
    cf[:, L["ONE"]] = 1.0
    cf[:, L["NEG"]:L["NEG"] + 4] = -1e30
    cf[:, L["MG"]:L["MG"] + NT] = 128.0 * np.arange(NT)[None, :]
    cf[:, L["JG"]:L["JG"] + NSLT] = np.arange(NSLT)[None, :]
    cb = np.zeros((128, 512), np.float32)
    cb[:, 0:128] = np.eye(128)
    s = np.arange(128)[:, None]
    c = np.arange(128)[None, :]
    cb[:, 128:256] = ((s // 32 == c // 32) & (s <= c)).astype(np.float32)
    cb[:, 256:384] = (s < c).astype(np.float32)
    cb[:, 384:512] = 1.0
    ci = (np.arange(NT)[None, :] * 128 + np.arange(128)[:, None]).astype(np.int32)
    return cf, cb.astype(ml_dtypes.bfloat16), ci


def build(NT, debug=False):
    T = NT * 128
    NSLT = 2 * NT + 32
    CAP = T
    NINV = 32 * CAP
    SST = (NSLT + NWB - 1) // NWB
    L = cf_layout(NT)
    nc = bass.Bass("TRN2", target_bir_lowering=False)
    P = Prog()

    def din(name, shape, dt):
        return nc.dram_tensor(name, list(shape), dt, kind="ExternalInput")

    def dscr(name, shape, dt, out=False):
        return nc.dram_tensor(name, list(shape), dt, kind="ExternalOutput" if out else "Internal")

    x_d = din("x", [T, 1024], F32)
    ccol_d = din("ccol", [128, 8], F32)
    wada_d = din("w_ada", [1024, 6144], F32)
    bada_d = din("b_ada", [1, 6144], F32)
    win_d = din("w_in", [1024, 2816], F32)
    wout_d = din("w_out", [1024, 1024], F32)
    wr_d = din("w_r", [1024, 36], F32)
    br_d = din("b_r", [1, 36], F32)
    ln1pre_d = din("ln1_pre", [1, 1024], F32)
    ln1post_d = din("ln1_post", [1, 1024], F32)
    ln2pre_d = din("ln2_pre", [1, 1024], F32)
    ln2post_d = din("ln2_post", [1, 1024], F32)
    sinks_d = din("sinks", [1, 8], F32)
    an_d = din("attn_norm", [1, 512], F32)
    hn_d = din("hgrn_norm", [1, 512], F32)
    lbc_d = din("lbc", [128, 8], F32)
    wg_d = din("w_gate", [32, 1024, 256], F32)
    wu_d = din("w_up", [32, 1024, 256], F32)
    wd_d = din("w_down", [32, 256, 1024], F32)
    cf_d = din("cf", [128, L["N"]], F32)
    cb_d = din("cb", [128, 512], BF16)
    ci_d = din("ci", [128, NT], I32)
    out_d = nc.dram_tensor("out", [T, 1024], F32, kind="ExternalOutput")
    mod_d = dscr("mod_s", [1, 6144], F32, out=debug)
    x1_d = dscr("x1_s", [T, 1024], F32, out=debug)
    h2_d = dscr("h2_s", [T, 1024], BF16, out=debug)
    inv_d = dscr("inv_s", [NINV, 1], I32)
    wall_d = dscr("wall_s", [4096, 6144], BF16)
    ys_d = dscr("ys_s", [NSLT * 128, 1024], BF16)
    cat_d = dscr("cat_s", [T, 1024], BF16, out=True) if debug else None

    st = ExitStack()
    pers = Arena(nc, SBUF_BASE)
    cf = pers.t("cf", [128, L["N"]], F32)
    cb = pers.t("cb", [128, 512], BF16)
    ci = pers.t("ci", [128, NT], I32)
    G2b = pers.t("G2b", [128, 1024], F32)
    EG = pers.t("EG", [128, 2 * NT], F32)
    WT = pers.t("WT", [128, 2 * NT], F32)
    RK = pers.t("RK", [128, 2 * NT], F32)
    POSI = pers.t("POSI", [128, 2 * NT], I32)
    Cb = pers.t("Cb", [128, 32], F32)
    IDXJ = pers.t("IDXJ", [128, NSLT], I32)
    WIDX = pers.t("WIDX", [128, NSLT], I32)
    dmy = pers.t("dmy", [128, 24], F32)
    ident = cb[:, 0:128]
    CM = cb[:, 128:256]
    Lst = cb[:, 256:384]
    ones_bf = cb[:, 384:512]

    def cfc(name, n=1, off=0):
        return cf[:, L[name] + off:L[name] + off + n]

    A = Arena(nc, pers.top)
    w_in_bf = A.t("w_in_bf", [128, 8, 2816], BF16)
    w_out_bf = A.t("w_out_bf", [128, 8, 1024], BF16)
    w_r_bf = A.t("w_r_bf", [128, 8, 36], BF16)
    GS1b = A.t("GS1b", [128, 1024], F32)
    SH1b = A.t("SH1b", [128, 1024], F32)
    G1b = A.t("G1b", [128, 1024], F32)
    GS2b = A.t("GS2b", [128, 1024], F32)
    SH2b = A.t("SH2b", [128, 1024], F32)
    ANb = A.t("ANb", [128, 512], F32)
    HNb = A.t("HNb", [128, 512], F32)
    BRb = A.t("BRb", [128, 36], F32)
    exsink = A.t("exsink", [128, 8], F32)
    lbc = A.t("lbc", [128, 8], F32)
    oml = A.t("oml", [128, 4], F32)
    homl = A.t("homl", [128, 4], F32)
    bhoml = A.t("bhoml", [128, 4], F32)
    XT = [A.t("xt%d" % i, [128, 1024], F32) for i in range(3)]
    tA = A.t("tA", [128, 1024], F32)
    tA2 = A.t("tA2", [128, 1024], F32)
    tA3 = A.t("tA3", [128, 1024], F32)
    junk = A.t("junk", [128, 1024], BF16)
    Hb = A.t("Hb", [128, 1024], BF16)
    CATb = A.t("CATb", [128, 1024], BF16)
    H2b = A.t("H2b", [128, 1024], BF16)
    hT = A.t("hT", [128, 8, 128], BF16)
    catT = A.t("catT", [128, 8, 128], BF16)
    h2T = A.t("h2T", [128, 8, 128], BF16)
    stage = [A.t("stage%d" % i, [128, 2048], BF16) for i in range(2)]
    wcv = [A.t("wcv%d" % i, [128, 2048], BF16) for i in range(2)]
    qT_sb = A.t("qT_sb", [128, 4, 128], BF16)
    kT = [A.t("kT%d" % i, [128, 128], BF16) for i in range(2)]
    vaug = [A.t("vaug%d" % i, [128, 2, 65], BF16) for i in range(2)]
    pe_sb = [A.t("pe_sb%d" % i, [128, 512], BF16) for i in range(4)]
    pT = [A.t("pT%d" % i, [128, 512], BF16) for i in range(4)]
    attn_o = A.t("attn_o", [128, 512], F32)
    qs = A.t("qs", [128, 512], F32)
    th = A.t("th", [128, 512], F32)
    sg = A.t("sg", [128, 512], F32)
    logf = A.t("logf", [128, 512], F32)
    bcs = A.t("bcs", [128, 512], F32)
    enb = A.t("enb", [128, 512], F32)
    dec = A.t("dec", [128, 16], F32)
    kdT = A.t("kdT", [128, 4, 128], BF16)
    qdT = A.t("qdT", [128, 4, 128], BF16)
    keT = A.t("keT", [128, 4, 128], BF16)
    ke_tm = A.t("ke_tm", [128, 4, 128], BF16)
    Vz = A.t("Vz", [128, 4, 512], BF16)
    v_bf = A.t("v_bf", [128, 512], BF16)
    gsn = A.t("gsn", [128, 512], F32)
    osb = A.t("osb", [128, 512], F32)
    zt = A.at("zt", [128, 512], I32, "osb")
    aTm = A.t("aTm", [128, 4, 128], BF16)
    Sb = A.t("Sb", [128, 4, 4, 128], BF16)
    Sprev = A.t("Sprev", [128, 4, 128], BF16)
    smA = A.t("smA", [128, 64], F32)
    smR = A.t("smR", [128, 160], F32)
    smU = A.t("smU", [128, 16], U32)
    smI = A.t("smI", [128, 2], I32)
    O01 = A.t("O01", [128, 64], BF16)
    modc = [A.t("modc%d" % i, [1, 256], F32) for i in range(2)]
    badac = [A.t("badac%d" % i, [1, 256], F32) for i in range(2)]
    ccol = A.t("ccol_s", [128, 8], F32)
    cact = A.t("cact", [128, 8], BF16)
    a_top = A.top

    B = Arena(nc, pers.top)
    wall = [B.t("wall%d" % i, [128, 6144], BF16) for i in range(NWB)]
    tokidx = [B.t("tokidx%d" % i, [128, 1], I32) for i in range(6)]
    xg = [B.t("xg%d" % i, [128, 1024], BF16) for i in range(3)]
    xgT = [B.t("xgT%d" % i, [128, 8, 128], BF16) for i in range(2)]
    sgm = B.t("sgm", [128, 256], F32)
    actT = [B.t("actT%d" % i, [128, 2, 128], BF16) for i in range(2)]
    ysb = [B.t("ysb%d" % i, [128, 1024], BF16) for i in range(2)]
    y0 = [B.t("y0_%d" % i, [128, 1024], BF16) for i in range(3)]
    y1 = [B.t("y1_%d" % i, [128, 1024], BF16) for i in range(3)]
    x1c = [B.t("x1c%d" % i, [128, 1024], F32) for i in range(3)]
    yc = B.t("yc", [128, 1024], F32)
    tC = B.t("tC", [128, 1024], F32)
    junkC = B.t("junkC", [128, 1024], BF16)
    smC = B.t("smC", [128, 16], F32)
    big = B.t("big", [128, 1024], F32)
    tend = B.t("tend", [128, 32], F32)
    ntl = B.t("ntl", [128, 32], F32)
    tst = B.t("tst", [128, 32], F32)
    pst = B.t("pst", [128, 32], F32)
    PB = B.t("PB", [128, 2 * NT], F32)
    EXJ = B.t("EXJ", [128, NSLT], F32)
    TSJ = B.t("TSJ", [128, NSLT], F32)
    IJF = B.t("IJF", [128, NSLT], F32)
    RJ = B.t("RJ", [128, NSLT], F32)
    print("SBUF tops: pers %d A %d B %d" % (pers.top, a_top, B.top))

    PSB = [st.enter_context(nc.psum_tensor("psb%d" % i, [128, 512], F32)) for i in range(8)]
    PSG = {"E": [0], "SC": [1, 2], "PV": [3], "HG": [4, 5], "LT": [6, 7], "ALL": [0, 1, 2, 3, 4, 5, 6, 7]}
    psring = {g: 0 for g in PSG}
    pserial = [0]

    def psnew(grp, tag="?"):
        banks = PSG[grp]
        i_ = banks[psring[grp] % len(banks)]
        psring[grp] += 1
        pserial[0] += 1
        PS_CUR["ps%d" % i_] = (pserial[0], tag)
        return PSB[i_], "ps%d" % i_

    def fsz(ap):
        n = 1
        for d in list(ap.shape)[1:]:
            n *= int(d)
        return n

    def dma(eng, out, in_, reads, writes):
        nbytes = fsz(out) * int(out.shape[0]) * dtsize(out.dtype)
        return P.add(eng, lambda e: e.dma_start(out=out, in_=in_), reads=reads, writes=writes, dma=True,
                     cost=0.15, dmat=nbytes / 250e3)

    def idma(out, out_off, in_, in_off, reads, writes, nbytes):
        return P.add("pool", lambda e: e.indirect_dma_start(out=out, out_offset=out_off, in_=in_, in_offset=in_off),
                     reads=reads, writes=writes, dma=True, cost=1.0, dmat=nbytes / 250e3)

    def act(out, in_, func, reads, writes, scale=None, bias=None, accum=None):
        kw = {}
        if scale is not None:
            kw["scale"] = scale
        if bias is not None:
            kw["bias"] = bias
        if accum is not None:
            kw["accum_out"] = accum
        tset = 18 if func in (AF.Silu, AF.Tanh) else (6 if func in (AF.Exp, AF.Ln) else (2 if func == AF.Sigmoid else None))
        return P.add("act", lambda e: e.activation(out=out, in_=in_, func=func, **kw), reads=reads, writes=writes,
                     cost=0.25 + fsz(in_) * 0.0009, tset=tset)

    def ecost(eng, n):
        if eng == "pool":
            return 0.2 + n * 0.002
        if eng == "act":
            return 0.25 + n * 0.0009
        return 0.1 + n * 0.0015

    def tt(eng, out, in0, in1, op, reads, writes):
        return P.add(eng, lambda e: e.tensor_tensor(out=out, in0=in0, in1=in1, op=op), reads=reads, writes=writes,
                     cost=ecost(eng, fsz(out)))

    def ts(eng, out, in0, s1, op0, reads, writes, s2=None, op1=None):
        c = ecost(eng, fsz(out))
        if op1 is None:
            return P.add(eng, lambda e: e.tensor_scalar(out=out, in0=in0, scalar1=s1, scalar2=None, op0=op0), reads=reads, writes=writes, cost=c)
        return P.add(eng, lambda e: e.tensor_scalar(out=out, in0=in0, scalar1=s1, scalar2=s2, op0=op0, op1=op1), reads=reads, writes=writes, cost=c)

    def stt(out, in0, scalar, in1, op0, op1, reads, writes, accum=None):
        kw = {"accum_out": accum} if accum is not None else {}
        return P.add("dve", lambda e: e.scalar_tensor_tensor(out=out, in0=in0, scalar=scalar, in1=in1, op0=op0, op1=op1, **kw), reads=reads, writes=writes,
                     cost=ecost("dve", fsz(out)))

    def cp(eng, out, in_, reads, writes):
        c = ecost(eng, fsz(out))
        if eng == "act":
            return P.add("act", lambda e: e.copy(out=out, in_=in_), reads=reads, writes=writes, cost=c)
        return P.add(eng, lambda e: e.tensor_copy(out=out, in_=in_), reads=reads, writes=writes, cost=c)

    def mm(out, lhsT, rhs, start, stop, reads, writes, tp=None, sgc=False):
        kw = {"tile_position": tp} if tp is not None else {}
        if sgc:
            kw["skip_group_check"] = True
        c = 0.035 + fsz(rhs) * 0.00042 * (4 if rhs.dtype == F32 else 1)
        return P.add("pe", lambda e: e.matmul(out, lhsT=lhsT, rhs=rhs, start=start, stop=stop, **kw), reads=reads, writes=writes, cost=c)

    def rstd(ss_ap, inv_n, out_ap, tmp_ap, key_in, key_tmp, key_out):
        act(tmp_ap, ss_ap, AF.Ln, [key_in, "cf"], [key_tmp], scale=inv_n, bias=cfc("EPS")[:ss_ap.shape[0], :])
        act(out_ap, tmp_ap, AF.Exp, [key_tmp], [key_out], scale=-0.5)

    def transpose_tm(src, src_key, dst, dst_key, grp, nchunk=8):
        pb, pk = psnew(grp, "tr")
        pbv = pb[:, :].bitcast(BF16)
        for j in range(nchunk):
            P.add("pe", lambda e, j=j: e.transpose(out=pbv[:, j * 128:(j + 1) * 128], in_=src[:, j * 128:(j + 1) * 128], identity=ident),
                  reads=[src_key, "cb"], writes=[pk], cost=0.09)
        cp("act", dst.rearrange("p k t -> p (k t)")[:, 0:nchunk * 128], pbv[:, 0:nchunk * 128], [pk], [dst_key])

    dma("sp", cf[:, :], cf_d.ap(), [], ["cf"])
    dma("sp", cb[:, :], cb_d.ap(), [], ["cb"])
    dma("sp", ci[:, :], ci_d.ap(), [], ["ci"])
    dma("sp", ccol[:, :], ccol_d.ap(), [], ["ccol"])
    dma("sp", lbc[:, :], lbc_d.ap(), [], ["lbc"])
    dma("sp", BRb[:, :], br_d.ap()[0:1, :].partition_broadcast(128), [], ["BRb"])
    dma("sp", exsink[:, :], sinks_d.ap()[0:1, :].partition_broadcast(128), [], ["exsink"])
    dma("sp", ANb[:, :], an_d.ap()[0:1, :].partition_broadcast(128), [], ["ANb"])
    dma("sp", HNb[:, :], hn_d.ap()[0:1, :].partition_broadcast(128), [], ["HNb"])
    dma("sp", GS1b[:, :], ln1pre_d.ap()[0:1, :].partition_broadcast(128), [], ["GS1b"])
    dma("sp", G1b[:, :], ln1post_d.ap()[0:1, :].partition_broadcast(128), [], ["G1b"])
    dma("sp", GS2b[:, :], ln2pre_d.ap()[0:1, :].partition_broadcast(128), [], ["GS2b"])
    dma("sp", G2b[:, :], ln2post_d.ap()[0:1, :].partition_broadcast(128), [], ["G2b"])
    P.add("pool", lambda e: e.memset(zt[:, :], 0), writes=["osb"])
    P.add("pool", lambda e: e.memset(Cb[:, :], 0.0), writes=["Cb"])
    P.add("pool", lambda e: e.memset(Sprev[:, :, :], 0.0), writes=["Sprev"])
    P.add("pool", lambda e: e.memset(dmy[:, :], 0.0), writes=["dmy"])
    for i in range(2):
        P.add("pool", lambda e, i=i: e.memset(vaug[i][:, :, :], 1.0), writes=["vaug%d" % i])
    inv_v = inv_d.ap().rearrange("(p f) o -> p (f o)", p=128)
    ninv_f = NINV // 128
    INV0_KEYS = [("inv0", c0) for c0 in range(0, ninv_f, 512)]
    INV_KEYS = [("inv", i, k_) for i in range(NT) for k_ in range(2)]
    H2_KEYS = [("h2_d", i) for i in range(NT)]
    YS_KEYS = [("ys_d", j) for j in range(NSLT)]
    WGU_KEYS = [("wgu_d", e_, c) for e_ in range(32) for c in range(2)]
    WDN_KEYS = [("wdn_d", e_) for e_ in range(32)]
    for c0 in range(0, ninv_f, 512):
        c1 = min(c0 + 512, ninv_f)
        dma("sp", inv_v[:, c0:c1], zt[:, 0:c1 - c0], ["osb"], [("inv0", c0)])
    act(exsink[:, :], exsink[:, :], AF.Exp, ["exsink"], ["exsink"])
    tt("dve", oml[:, :], lbc[:, 4:8], lbc[:, 0:4], ALU.subtract, ["lbc"], ["oml"])
    act(oml[:, :], oml[:, :], AF.Sigmoid, ["oml"], ["oml"])
    ts("dve", homl[:, :], oml[:, :], 0.5, ALU.mult, ["oml"], ["homl"])
    ts("dve", bhoml[:, :], oml[:, :], -0.5, ALU.mult, ["oml"], ["bhoml"], s2=1.0, op1=ALU.add)

    sidx = [0]

    def stage_load(src_ap, ncols_view):
        s = sidx[0] % 2
        sidx[0] += 1
        dma("sp", ncols_view(stage[s]), src_ap, [], ["stage%d" % s])
        return s

    def dmac(out, in_, reads, writes, nbytes):
        return P.add("pool", lambda e: e.dma_start(out=out, in_=in_), reads=reads, writes=writes, dma=True,
                     cost=1.0, dmat=nbytes / 250e3)
    CW = int(_os.environ.get("CAST_CW", "512"))
    WIN_KEYS, WOUT_KEYS = [], []
    for k in range(8):
        for c0 in range(0, 2816, CW):
            c1 = min(c0 + CW, 2816)
            dmac(w_in_bf[:, k, c0:c1], win_d.ap()[k * 128:(k + 1) * 128, c0:c1], [], [("w_in_c", k, c0)], 128 * (c1 - c0) * 4)
            WIN_KEYS.append(("w_in_c", k, c0))
    for k in range(8):
        for c0 in range(0, 1024, CW):
            c1 = min(c0 + CW, 1024)
            dmac(w_out_bf[:, k, c0:c1], wout_d.ap()[k * 128:(k + 1) * 128, c0:c1], [], [("w_out_c", k, c0)], 128 * (c1 - c0) * 4)
            WOUT_KEYS.append(("w_out_c", k, c0))
    dmac(w_r_bf[:, :, :], wr_d.ap().rearrange("(k p) n -> p k n", p=128), [], ["w_r_bf"], 128 * 288 * 4)
    P.add("pe", None, reads=WIN_KEYS, writes=["w_in_bf"], cost=0.01)
    P.add("pe", None, reads=WOUT_KEYS, writes=["w_out_bf"], cost=0.01)

    act(cact[:, :], ccol[:, :], AF.Silu, ["ccol"], ["cact"])
    wada_v = wada_d.ap().rearrange("(k p) n -> p k n", p=128)
    NCH = 24
    for n in range(NCH):
        s_ = n % 2
        sv = stage[s_][:, :].rearrange("p (k n) -> p k n", n=256)
        dmac(sv, wada_v[:, :, n * 256:(n + 1) * 256], [], ["stage%d" % s_], 1024 * 256 * 4)
        pb, pk = psnew("LT", "ada")
        for k in range(8):
            mm(pb[0:1, 0:256], cact[:, k:k + 1], sv[:, k, :], k == 0, k == 7, ["cact", "stage%d" % s_], [pk])
        r_ = n % 2
        dma("sp", badac[r_][0:1, :], bada_d.ap()[0:1, n * 256:(n + 1) * 256], [], ["badac%d" % r_])
        tt("dve", modc[r_][0:1, :], pb[0:1, 0:256], badac[r_][0:1, :], ALU.add, [pk, "badac%d" % r_], ["modc%d" % r_])
        dma("sp", mod_d.ap()[0:1, n * 256:(n + 1) * 256], modc[r_][0:1, :], ["modc%d" % r_], [("mod_d", n)])
    MOD_KEYS = [("mod_d", n) for n in range(NCH)]

    def modb(j):
        return mod_d.ap()[0:1, j * 1024:(j + 1) * 1024].partition_broadcast(128)

    def modk(j):
        return [("mod_d", n) for n in range(4 * j, 4 * j + 4)]
    dma("sp", SH1b[:, :], modb(0), modk(0), ["SH1b"])
    dma("sp", tA[:, :], modb(1), modk(1), ["tA"])
    stt(GS1b[:, :], tA[:, :], 1.0, GS1b[:, :], ALU.add, ALU.mult, ["tA", "GS1b"], ["GS1b"])
    dma("sp", XT[1][:, :], modb(2), modk(2), ["xt1"])
    tt("dve", G1b[:, :], XT[1][:, :], G1b[:, :], ALU.mult, ["xt1", "G1b"], ["G1b"])
    dma("sp", SH2b[:, :], modb(3), modk(3), ["SH2b"])
    dma("sp", XT[2][:, :], modb(4), modk(4), ["xt2"])
    stt(GS2b[:, :], XT[2][:, :], 1.0, GS2b[:, :], ALU.add, ALU.mult, ["xt2", "GS2b"], ["GS2b"])
    dma("sp", XT[1][:, :], modb(5), modk(5), ["xt1"])
    tt("dve", G2b[:, :], XT[1][:, :], G2b[:, :], ALU.mult, ["xt1", "G2b"], ["G2b"])

    wgu_dv = wall_d.ap()[:, 0:4096].rearrange("r (k n) -> r k n", n=512)
    wdn_dv = wall_d.ap()[:, 4096:6144].rearrange("r (f n) -> r f n", n=1024)
    jobs = []
    for e_ in range(32):
        jobs.append(("g", e_))
        jobs.append(("u", e_))
        jobs.append(("d", e_))
    wcvi = [0]

    wcvi = [0]
    CONV_GATE = [[]]

    def conv_job(job):
        kind, e_ = job
        w = wcvi[0] % 2
        wcvi[0] += 1
        if kind in ("g", "u"):
            src = (wg_d if kind == "g" else wu_d).ap()[e_].rearrange("(k p) n -> p k n", p=128)
            c0 = 0 if kind == "g" else 256
            dmac(wcv[w][:, :].rearrange("p (k n) -> p k n", n=256), src, CONV_GATE[0], ["wcv%d" % w], 1024 * 256 * 4)
            dma("sp", wgu_dv[e_ * 128:(e_ + 1) * 128, :, c0:c0 + 256], wcv[w][:, :].rearrange("p (k n) -> p k n", n=256),
                ["wcv%d" % w], [("wgu_d", e_, 0 if kind == "g" else 1)])
        else:
            src = wd_d.ap()[e_].rearrange("(f p) n -> p f n", p=128)
            dmac(wcv[w][:, :].rearrange("p (f n) -> p f n", n=1024), src, CONV_GATE[0], ["wcv%d" % w], 256 * 1024 * 4)
            dma("sp", wdn_dv[e_ * 128:(e_ + 1) * 128, :, :], wcv[w][:, :].rearrange("p (f n) -> p f n", n=1024),
                ["wcv%d" % w], [("wdn_d", e_)])

    def tileA(i):
        par = i % 2
        xt = XT[i % 3]
        xk = "xt%d" % (i % 3)
        rows = slice(i * 128, (i + 1) * 128)
        dma("sp", xt[:, :], x_d.ap()[rows, :], [], [xk])
        act(junk[:, :], xt[:, :], AF.Square, [xk], ["ss1"], accum=smA[:, 0:1])
        rstd(smA[:, 0:1], 1.0 / 1024, smA[:, 2:3], smA[:, 1:2], "ss1", "ln1", "rs1")
        stt(tA[:, :], xt[:, :], smA[:, 2:3], GS1b[:, :], ALU.mult, ALU.mult, [xk, "rs1", "GS1b"], ["tA"])
        tt("pool", Hb[:, :], tA[:, :], SH1b[:, :], ALU.add, ["tA", "SH1b"], ["Hb"])
        transpose_tm(Hb, "Hb", hT, "hT", "E")
        pq, kq = psnew("E", "q")
        for m in range(4):
            for k in range(8):
                mm(pq[:, m * 128:(m + 1) * 128], w_in_bf[:, k, m * 128:(m + 1) * 128], hT[:, k, :], k == 0, k == 7,
                   ["w_in_bf", "hT"], [kq])
        act(qT_sb.rearrange("p m t -> p (m t)"), pq[:, 0:512], AF.Identity, [kq], ["qT_sb"], scale=0.125)
        pkv, kkv = psnew("E", "kv")
        for k in range(8):
            mm(pkv[:, 0:128], w_in_bf[:, k, 512:640], hT[:, k, :], k == 0, k == 7, ["w_in_bf", "hT"], [kkv])
        for k in range(8):
            mm(pkv[:, 128:256], hT[:, k, :], w_in_bf[:, k, 640:768], k == 0, k == 7, ["w_in_bf", "hT"], [kkv])
        cp("dve", kT[par][:, :], pkv[:, 0:128], [kkv], ["kT%d" % par])
        cp("dve", vaug[par][:, :, 0:64], pkv[:, 128:256].rearrange("p (g d) -> p g d", d=64), [kkv], ["vaug%d" % par])
        for g in range(2):
            kbs = []
            if i > 0:
                kbs.append((1 - par, 0))
            kbs.append((par, 1))
            pvb, kvb = psnew("PV", "pv")
            for n, (bp, kb) in enumerate(kbs):
                ring = (2 * g + n) % 4
                psc, ksc = psnew("SC", "sc")
                mm(psc[:, :], kT[bp][64 * g:64 * g + 64, :], qT_sb[64 * g:64 * g + 64, :, :].rearrange("p m t -> p (m t)"),
                   True, True, ["kT%d" % bp, "qT_sb"], [ksc], tp=(64 * g, 0))
                act(pe_sb[ring][:, :], psc[:, :], AF.Exp, [ksc], ["pe_sb%d" % ring])
                c0 = L["E"] + (kb * 2 + g) * 512
                tt("pool", pT[ring][:, :], pe_sb[ring][:, :], cf[:, c0:c0 + 512], ALU.mult, ["pe_sb%d" % ring, "cf"], ["pT%d" % ring])
                for r in range(4):
                    mm(pvb[:, r * 65:(r + 1) * 65], pT[ring][:, r * 128:(r + 1) * 128], vaug[bp][:, g, :], n == 0 and r == 0,
                       n == len(kbs) - 1, ["pT%d" % ring, "vaug%d" % bp], [kvb], sgc=True)
            pv = pvb[:, 0:260].rearrange("p (r c) -> p r c", c=65)
            tt("dve", smA[:, 8:12], pv[:, :, 64], exsink[:, 4 * g:4 * g + 4], ALU.add, [kvb, "exsink"], ["den"])
            P.add("dve", lambda e: e.reciprocal(out=smA[:, 12:16], in_=smA[:, 8:12]), reads=["den"], writes=["rden"])
            tt("dve", attn_o[:, g * 256:(g + 1) * 256].rearrange("p (r d) -> p r d", d=64), pv[:, :, 0:64],
               smA[:, 12:16].unsqueeze(2).to_broadcast([128, 4, 64]), ALU.mult, [kvb, "rden"], ["attn_o"])
        act(junk[:, 0:512], attn_o[:, :], AF.Square, ["attn_o"], ["ssa"], accum=smA[:, 16:17])
        rstd(smA[:, 16:17], 1.0 / 512, smA[:, 18:19], smA[:, 17:18], "ssa", "lna", "rsa")
        stt(CATb[:, 0:512], attn_o[:, :], smA[:, 18:19], ANb[:, :], ALU.mult, ALU.mult, ["attn_o", "rsa", "ANb"], ["CATb"])
        phq, khq = psnew("HG", "hq")
        for h in range(4):
            for k in range(8):
                mm(phq[:, h * 128:(h + 1) * 128], w_in_bf[:, k, 768 + h * 128:768 + (h + 1) * 128], hT[:, k, :], k == 0, k == 7,
                   ["w_in_bf", "hT"], [khq])
        act(qs[:, :], phq[:, :], AF.Silu, [khq], ["qs"])
        phf, khf = psnew("HG", "hf")
        for h in range(4):
            for k in range(8):
                mm(phf[:, h * 128:(h + 1) * 128], w_in_bf[:, k, 1280 + h * 128:1280 + (h + 1) * 128], hT[:, k, :],
                   k == 0, k == 7, ["w_in_bf", "hT"], [khf])
        act(th[:, :], phf[:, :], AF.Tanh, [khf], ["th"], scale=0.5)
        phi, khi = psnew("HG", "hi")
        for k in range(8):
            mm(phi[:, :], hT[:, k, :], w_in_bf[:, k, 1792:2304], k == 0, k == 7, ["w_in_bf", "hT"], [khi])
        tt("dve", Vz[:, :, :], phi[:, :].unsqueeze(1).to_broadcast([128, 4, 512]),
           cfc("MJ", 4).unsqueeze(2).to_broadcast([128, 4, 512]), ALU.mult, [khi, "cf"], ["Vz"])
        cp("act", v_bf[:, :], phi[:, :], [khi], ["v_bf"])
        phg, khg = psnew("HG", "hg")
        for k in range(8):
            mm(phg[:, :], hT[:, k, :], w_in_bf[:, k, 2304:2816], k == 0, k == 7, ["w_in_bf", "hT"], [khg])
        act(gsn[:, :], phg[:, :], AF.Silu, [khg], ["gsn"])
        ts("pool", sg[:, :], th[:, :], -0.5, ALU.mult, ["th"], ["sg"], s2=0.5, op1=ALU.add)
        for h in range(4):
            act(logf[:, h * 128:(h + 1) * 128], th[:, h * 128:(h + 1) * 128], AF.Ln, ["th", "homl", "bhoml"], ["logf"],
                scale=homl[:, h:h + 1], bias=bhoml[:, h:h + 1])
        P.add("dve", lambda e: e.tensor_tensor_scan(out=bcs[:, :], data0=cfc("RM", 512), data1=logf[:, :], initial=0.0,
                                                    op0=ALU.mult, op1=ALU.add), reads=["logf", "cf"], writes=["bcs"], cost=1.15)
        act(logf[:, :], bcs[:, :], AF.Exp, ["bcs"], ["logf"])
        act(enb[:, :], bcs[:, :], AF.Exp, ["bcs"], ["enb"], scale=-1.0)
        act(dec[:, :], bcs[:, :].rearrange("p (c t) -> p c t", t=32)[:, :, 31], AF.Exp, ["bcs"], ["dec"])
        for h in range(4):
            stt(kdT[:, h, :], sg[:, h * 128:(h + 1) * 128], oml[:, h:h + 1], enb[:, h * 128:(h + 1) * 128], ALU.mult, ALU.mult,
                ["sg", "oml", "enb"], ["kdT"])
        tt("pool", qdT.rearrange("p h t -> p (h t)"), qs[:, :], logf[:, :], ALU.mult, ["qs", "logf"], ["qdT"])
        tt("pool", keT.rearrange("p h (c t) -> p (h c) t", t=32), kdT.rearrange("p h (c t) -> p (h c) t", t=32),
           dec[:, :].unsqueeze(2).to_broadcast([128, 16, 32]), ALU.mult, ["kdT", "dec"], ["keT"])
        tt("pool", gsn[:, :], gsn[:, :], HNb[:, :], ALU.mult, ["gsn", "HNb"], ["gsn"])
        pkt, kkt = psnew("HG", "ket")
        pktv = pkt[:, :].bitcast(BF16)
        for h in range(4):
            P.add("pe", lambda e, h=h: e.transpose(out=pktv[:, h * 128:(h + 1) * 128], in_=keT[:, h, :], identity=ident),
                  reads=["keT", "cb"], writes=[kkt], cost=0.09)
        cp("act", ke_tm.rearrange("p h k -> p (h k)"), pktv[:, 0:512], [kkt], ["ke_tm"])
        pat, kat = psnew("HG", "aT")
        for h in range(4):
            mm(pat[:, h * 128:(h + 1) * 128], kdT[:, h, :], qdT[:, h, :], True, True, ["kdT", "qdT"], [kat])
        tt("dve", aTm[:, :, :], pat[:, :].rearrange("p (h c) -> p h c", c=128), CM.unsqueeze(1).to_broadcast([128, 4, 128]),
           ALU.mult, [kat, "cb"], ["aTm"])
        for h in range(4):
            pv_, pk = psnew("HG", "U")
            mm(pv_[:, :].rearrange("p (j v) -> p j v", v=128), ke_tm[:, h, :], Vz[:, :, h * 128:(h + 1) * 128], True, True,
               ["ke_tm", "Vz"], [pk])
            for j in range(4):
                sp_ap = Sprev[:, h, :] if j == 0 else Sb[:, h, j - 1, :]
                sp_key = "Sprev" if j == 0 else ("Sb", h, j - 1)
                stt(Sb[:, h, j, :], sp_ap, dec[:, 4 * h + j:4 * h + j + 1], pv_[:, j * 128:(j + 1) * 128], ALU.mult, ALU.add,
                    [sp_key, "dec", pk], [("Sb", h, j)])
        pho, kho = psnew("HG", "o")
        for h in range(4):
            for j in range(4):
                sp_ap = Sprev[:, h, :] if j == 0 else Sb[:, h, j - 1, :]
                sp_key = "Sprev" if j == 0 else ("Sb", h, j - 1)
                o_ap = pho[32 * j:32 * j + 32, h * 128:(h + 1) * 128]
                mm(o_ap, aTm[:, h, 32 * j:32 * j + 32], v_bf[:, h * 128:(h + 1) * 128], True, False, ["aTm", "v_bf"], [kho],
                   tp=(0, 32 * j))
                mm(o_ap, qdT[:, h, 32 * j:32 * j + 32], sp_ap, False, True, ["qdT", sp_key], [kho], tp=(0, 32 * j))
        P.add("pool", lambda e: e.tensor_copy(out=Sprev[:, :, :], in_=Sb[:, :, 3, :]),
              reads=[("Sb", h, 3) for h in range(4)], writes=["Sprev"])
        cp("act", osb[:, :], pho[:, :], [kho], ["osb"])
        act(th[:, :], osb[:, :], AF.Square, ["osb"], ["th"])
        P.add("dve", lambda e: e.tensor_reduce(out=smA[:, 20:24], in_=th[:, :].rearrange("p (h v) -> p h v", v=128), axis=AX.X, op=ALU.add),
              reads=["th"], writes=["ssh"])
        rstd(smA[:, 20:24], 1.0 / 128, smA[:, 28:32], smA[:, 24:28], "ssh", "lnh", "rsh")
        tt("dve", sg[:, :].rearrange("p (h v) -> p h v", v=128), osb[:, :].rearrange("p (h v) -> p h v", v=128),
           smA[:, 28:32].unsqueeze(2).to_broadcast([128, 4, 128]), ALU.mult, ["osb", "rsh"], ["sg"])
        tt("pool", CATb[:, 512:1024], sg[:, :], gsn[:, :], ALU.mult, ["sg", "gsn"], ["CATb"])
        if debug:
            dma("sp", cat_d.ap()[rows, :], CATb[:, :], ["CATb"], [("cat_d", i)])
        transpose_tm(CATb, "CATb", catT, "catT", "LT")
        pmx = []
        for n in range(2):
            pm_, km_ = psnew("LT", "mix")
            pmx.append((pm_, km_))
            for k in range(8):
                mm(pm_[:, :], catT[:, k, :], w_out_bf[:, k, n * 512:(n + 1) * 512], k == 0, k == 7,
                   ["catT", "w_out_bf"], [km_])
            act(junk[:, n * 512:(n + 1) * 512], pm_[:, :], AF.Square, [km_], ["ssm%d" % n], accum=smA[:, 40 + n:41 + n])
        tt("dve", smA[:, 32:33], smA[:, 40:41], smA[:, 41:42], ALU.add, ["ssm0", "ssm1"], ["ssm"])
        rstd(smA[:, 32:33], 1.0 / 1024, smA[:, 34:35], smA[:, 33:34], "ssm", "lnm", "rsm")
        for n in range(2):
            pm_, km_ = pmx[n]
            stt(tA2[:, n * 512:(n + 1) * 512], pm_[:, :], smA[:, 34:35], G1b[:, n * 512:(n + 1) * 512], ALU.mult, ALU.mult,
                [km_, "rsm", "G1b"], ["tA2"])
        tt("pool", xt[:, :], tA2[:, :], xt[:, :], ALU.add, ["tA2", xk], [xk])
        dma("sp", x1_d.ap()[rows, :], xt[:, :], [xk], [("x1_d", i)])
        act(junk[:, :], xt[:, :], AF.Square, [xk], ["ss2"], accum=smA[:, 36:37])
        rstd(smA[:, 36:37], 1.0 / 1024, smA[:, 38:39], smA[:, 37:38], "ss2", "ln2", "rs2")
        stt(tA3[:, :], xt[:, :], smA[:, 38:39], GS2b[:, :], ALU.mult, ALU.mult, [xk, "rs2", "GS2b"], ["tA3"])
        tt("pool", H2b[:, :], tA3[:, :], SH2b[:, :], ALU.add, ["tA3", "SH2b"], ["H2b"])
        dma("sp", h2_d.ap()[rows, :], H2b[:, :], ["H2b"], [("h2_d", i)])
        transpose_tm(H2b, "H2b", h2T, "h2T", "LT")
        psR, kpr = psnew("LT", "lg")
        for k in range(8):
            mm(psR[:, 0:36], h2T[:, k, :], w_r_bf[:, k, :], k == 0, k == 7, ["h2T", "w_r_bf"], [kpr])
        R_ = smR
        g8, le = R_[:, 0:8], R_[:, 8:40]
        gm, gif, ngm, gsum, gw = R_[:, 40:48], R_[:, 48:49], R_[:, 49:50], R_[:, 50:51], R_[:, 51:52]
        ohg, esel, em, eif = R_[:, 52:56], R_[:, 56:64], R_[:, 64:72], R_[:, 72:74]
        dd, ex, den2, w0 = R_[:, 74:75], R_[:, 75:76], R_[:, 76:77], R_[:, 77:78]
        t48, r01, sif, gex = R_[:, 80:112], R_[:, 112:144], R_[:, 144:146], R_[:, 148:152]
        cp("dve", g8[:, 4:8], cfc("NEG", 4), ["cf"], ["g8"])
        tt("dve", g8[:, 0:4], psR[:, 0:4], BRb[:, 0:4], ALU.add, [kpr, "BRb"], ["g8"])
        tt("dve", le, psR[:, 4:36], BRb[:, 4:36], ALU.add, [kpr, "BRb"], ["le"])
        P.add("dve", lambda e: e.max(out=gm, in_=g8), reads=["g8"], writes=["gm"])
        P.add("dve", lambda e: e.max_index(out=smU[:, 0:8], in_max=gm, in_values=g8), reads=["g8", "gm"], writes=["gi"])
        cp("dve", gif, smU[:, 0:1], ["gi"], ["gif"])
        ts("dve", ngm, gm[:, 0:1], -1.0, ALU.mult, ["gm"], ["ngm"])
        act(gex, g8[:, 0:4], AF.Exp, ["g8", "ngm"], ["gex", "gsum"], bias=ngm, accum=gsum)
        P.add("dve", lambda e: e.reciprocal(out=gw, in_=gsum), reads=["gsum"], writes=["gw"])
        ts("dve", ohg, cfc("IO32", 4), gif, ALU.is_equal, ["cf", "gif"], ["ohg"])
        tt("dve", t48.rearrange("p (g e) -> p g e", e=8), le.rearrange("p (g e) -> p g e", e=8),
           ohg.unsqueeze(2).to_broadcast([128, 4, 8]), ALU.mult, ["le", "ohg"], ["t48"])
        P.add("dve", lambda e: e.tensor_reduce(out=esel, in_=t48.rearrange("p (g e) -> p e g", e=8), axis=AX.X, op=ALU.add),
              reads=["t48"], writes=["esel"])
        P.add("dve", lambda e: e.max(out=em, in_=esel), reads=["esel"], writes=["em"])
        P.add("dve", lambda e: e.max_index(out=smU[:, 8:16], in_max=em, in_values=esel), reads=["esel", "em"], writes=["ei"])
        cp("dve", eif, smU[:, 8:10], ["ei"], ["eif"])
        tt("dve", dd, em[:, 1:2], em[:, 0:1], ALU.subtract, ["em"], ["dd"])
        act(ex, dd, AF.Exp, ["dd"], ["ex"])
        ts("dve", den2, ex, 1.0, ALU.add, ["ex"], ["den2"])
        P.add("dve", lambda e: e.reciprocal(out=w0, in_=den2), reads=["den2"], writes=["w0"])
        tt("dve", WT[:, 2 * i:2 * i + 1], w0, gw, ALU.mult, ["w0", "gw"], ["WT"])
        stt(WT[:, 2 * i + 1:2 * i + 2], ex, w0, gw, ALU.mult, ALU.mult, ["ex", "w0", "gw", "WT"], ["WT"])
        EG2 = EG[:, 2 * i:2 * i + 2]
        RK2 = RK[:, 2 * i:2 * i + 2]
        stt(EG2, gif.to_broadcast([128, 2]), 8.0, eif, ALU.mult, ALU.add, ["gif", "eif"], ["EG"])
        for k_ in range(2):
            ts("dve", O01[:, 32 * k_:32 * k_ + 32], cfc("IO32", 32), EG[:, 2 * i + k_:2 * i + k_ + 1], ALU.is_equal, ["cf", "EG"], ["O%d" % k_])
        O0, O1 = O01[:, 0:32], O01[:, 32:64]
        psR, kpr = psnew("LT", "rk")
        mm(psR[:, 64:96], Lst, O0, True, True, ["cb", "O0"], [kpr])
        mm(psR[:, 96:128], Lst, O1, True, False, ["cb", "O1"], [kpr])
        mm(psR[:, 96:128], ones_bf, O0, False, True, ["cb", "O0"], [kpr])
        mm(psR[:, 128:160], ones_bf, O0, True, False, ["cb", "O0"], [kpr])
        mm(psR[:, 128:160], ones_bf, O1, False, True, ["cb", "O1"], [kpr])
        for k_ in range(2):
            tt("dve", r01, psR[:, 64 + 32 * k_:96 + 32 * k_], Cb[:, :], ALU.add, [kpr, "Cb"], ["r01"])
            stt(t48, r01, 1.0, O01[:, 32 * k_:32 * k_ + 32], ALU.mult, ALU.mult, ["r01", "O%d" % k_], ["t48", "RK"],
                accum=RK[:, 2 * i + k_:2 * i + k_ + 1])
        tt("dve", Cb[:, :], Cb[:, :], psR[:, 128:160], ALU.add, ["Cb", kpr], ["Cb"])
        stt(sif, EG2, float(CAP), RK2, ALU.mult, ALU.add, ["EG", "RK"], ["sif"])
        cp("dve", smI[:, 0:2], sif, ["sif"], ["smI"])
        for k_ in range(2):
            P.add("pool", lambda e, k_=k_: e.indirect_dma_start(out=inv_d.ap(), out_offset=bass.IndirectOffsetOnAxis(ap=smI[:, k_:k_ + 1], axis=0),
                                                                in_=ci[:, i:i + 1], in_offset=None),
                  reads=["smI", "ci"] + INV0_KEYS, writes=[("inv", i, k_)], dma=True, cost=1.0, dmat=0.05)

    njob = 0
    if _os.environ.get("NOCONV") == "1":
        jobs = []
    for i in range(NT):
        tileA(i)
        CONV_GATE[0] = [("x1_d", i)]
        tgt = (len(jobs) * (i + 1) + NT - 1) // NT
        while njob < min(tgt, len(jobs)):
            conv_job(jobs[njob])
            njob += 1
    while njob < len(jobs):
        conv_job(jobs[njob])
        njob += 1

    P.barrier([
        ("act", lambda e: e.copy(out=dmy[:, 0:8], in_=dmy[:, 0:8])),
        ("dve", lambda e: e.memset(dmy[:, 8:16], 0.0)),
        ("pool", lambda e: e.memset(dmy[:, 16:24], 0.0)),
    ])

    NTM = NT
    per = min(32, 1024 // NTM)
    for e0 in range(0, 32, per):
        bv = big[:, 0:per * NTM].rearrange("p (e m) -> p e m", m=NTM)
        tt("dve", bv, Cb[:, e0:e0 + per].unsqueeze(2).to_broadcast([128, per, NTM]),
           cfc("MG", NTM).unsqueeze(1).to_broadcast([128, per, NTM]), ALU.is_gt, ["Cb", "cf"], ["big"])
        P.add("dve", lambda e, e0=e0, bv=bv: e.tensor_reduce(out=ntl[:, e0:e0 + per], in_=bv, axis=AX.X, op=ALU.add),
              reads=["big"], writes=["ntl"])
    P.add("dve", lambda e: e.memset(big[:, 0:32], 1.0), reads=["ntl"], writes=["big"])
    P.add("dve", lambda e: e.tensor_tensor_scan(out=tend[:, :], data0=big[:, 0:32], data1=ntl[:, :], initial=0.0, op0=ALU.mult, op1=ALU.add),
          reads=["big", "ntl"], writes=["tend"])
    tt("dve", tst[:, :], tend[:, :], ntl[:, :], ALU.subtract, ["tend", "ntl"], ["tst"])
    ts("dve", pst[:, :], tst[:, :], 128.0, ALU.mult, ["tst"], ["pst"])
    for c0 in range(0, NSLT, 32):
        n_ = min(32, NSLT - c0)
        bv = big[:, 0:n_ * 32].rearrange("p (c e) -> p c e", e=32)
        tt("dve", bv, tend[:, :].unsqueeze(1).to_broadcast([128, n_, 32]), cfc("JG", n_, c0).unsqueeze(2).to_broadcast([128, n_, 32]),
           ALU.is_le, ["tend", "cf"], ["big"])
        P.add("dve", lambda e, c0=c0, n_=n_, bv=bv: e.tensor_reduce(out=EXJ[:, c0:c0 + n_], in_=bv, axis=AX.X, op=ALU.add),
              reads=["big"], writes=["EXJ"])
    ts("dve", EXJ[:, :], EXJ[:, :], 31.0, ALU.min, ["EXJ"], ["EXJ"])
    P.add("dve", lambda e: e.memset(TSJ[:, 0:1], 0.0), reads=[], writes=["TSJ"])
    tt("dve", TSJ[:, 1:NSLT], EXJ[:, 1:NSLT], EXJ[:, 0:NSLT - 1], ALU.is_equal, ["EXJ"], ["TSJ"])
    P.add("dve", lambda e: e.tensor_tensor_scan(out=RJ[:, :], data0=TSJ[:, :], data1=TSJ[:, :], initial=0.0, op0=ALU.mult, op1=ALU.add),
          reads=["TSJ"], writes=["RJ"], cost=0.5)
    ts("dve", IJF[:, :], RJ[:, :], 128.0, ALU.mult, ["RJ"], ["IJF"], s2=cfc("PIDX"), op1=ALU.add)
    stt(IJF[:, :], EXJ[:, :], float(CAP), IJF[:, :], ALU.mult, ALU.add, ["EXJ", "IJF"], ["IJF"])
    ts("dve", IJF[:, :], IJF[:, :], float(NINV - 1), ALU.min, ["IJF"], ["IJF"])
    cp("dve", IDXJ[:, :], IJF[:, :], ["IJF"], ["IDXJ"])
    ts("dve", IJF[:, :], EXJ[:, :], 128.0, ALU.mult, ["EXJ", "IDXJ"], ["IJF"], s2=cfc("PIDX"), op1=ALU.add)
    for r_ in range(1, NWB):
        if r_ * SST < NSLT:
            P.add("dve", lambda e, r_=r_: e.memset(TSJ[:, r_ * SST:r_ * SST + 1], 0.0), reads=["RJ"], writes=["TSJ"])
    stt(IJF[:, 1:NSLT], TSJ[:, 1:NSLT], 1.0e6, IJF[:, 1:NSLT], ALU.mult, ALU.add, ["TSJ", "IJF"], ["IJF"])
    cp("dve", WIDX[:, :], IJF[:, :], ["IJF"], ["WIDX"])

    def fetchT(j, r6):
        P.add("pool", lambda e: e.indirect_dma_start(out=tokidx[r6][:, 0:1], out_offset=None, in_=inv_d.ap(),
                                                     in_offset=bass.IndirectOffsetOnAxis(ap=IDXJ[:, j:j + 1], axis=0)),
              reads=["IDXJ"] + INV_KEYS + INV0_KEYS, writes=["tokidx%d" % r6], dma=True, cost=1.0, dmat=0.05)

    def fetchX(j, r3, r6):
        P.add("pool", lambda e: e.indirect_dma_start(out=xg[r3][:, :], out_offset=None, in_=h2_d.ap(),
                                                     in_offset=bass.IndirectOffsetOnAxis(ap=tokidx[r6][:, 0:1], axis=0)),
              reads=["tokidx%d" % r6] + H2_KEYS, writes=["xg%d" % r3], dma=True, cost=1.0, dmat=1.05)

    def fetchW(j, rw):
        P.add("pool", lambda e: e.indirect_dma_start(out=wall[rw][:, :], out_offset=None, in_=wall_d.ap(),
                                                     in_offset=bass.IndirectOffsetOnAxis(ap=WIDX[:, j:j + 1], axis=0),
                                                     bounds_check=BC_REG[0], oob_is_err=False),
              reads=["WIDX"] + WGU_KEYS + WDN_KEYS, writes=["wall%d" % rw], dma=True, cost=1.0, dmat=3.0)

    def tileB(j, rw, r3, r2):
        gT = xgT[r2]
        ptb, ktb = psnew("ALL", "trB")
        ptbv = ptb[:, :].bitcast(BF16)
        for c in range(8):
            P.add("pe", lambda e, c=c: e.transpose(out=ptbv[:, c * 128:(c + 1) * 128], in_=xg[r3][:, c * 128:(c + 1) * 128], identity=ident),
                  reads=["xg%d" % r3, "cb"], writes=[ktb], cost=0.09)
        cp("act", gT.rearrange("p k t -> p (k t)"), ptbv[:, :], [ktb], ["xgT%d" % r2])
        wv = wall[rw][:, 0:4096].rearrange("p (k n) -> p k n", n=512)
        ps_gu, kgu = psnew("ALL", "gu")
        for q_ in range(4):
            for k in range(8):
                mm(ps_gu[:, q_ * 128:(q_ + 1) * 128], wv[:, k, q_ * 128:(q_ + 1) * 128], gT[:, k, :], k == 0, k == 7,
                   ["wall%d" % rw, "xgT%d" % r2], [kgu])
        act(sgm[:, :], ps_gu[:, 0:256], AF.Silu, [kgu], ["sgm"])
        tt("dve", actT[r2].rearrange("p f t -> p (f t)"), sgm[:, :], ps_gu[:, 256:512], ALU.mult, ["sgm", kgu], ["actT%d" % r2])
        dv = wall[rw][:, 4096:6144].rearrange("p (f n) -> p f n", n=1024)
        for n in range(2):
            pd_, kd_ = psnew("ALL", "dn")
            for f in range(2):
                mm(pd_[:, :], actT[r2][:, f, :], dv[:, f, n * 512:(n + 1) * 512], f == 0, f == 1,
                   ["actT%d" % r2, "wall%d" % rw], [kd_])
            cp("act" if n == 0 else "dve", ysb[r2][:, n * 512:(n + 1) * 512], pd_[:, :], [kd_], ["ysb%d" % r2])
        dma("sp", ys_d.ap()[j * 128:(j + 1) * 128, :], ysb[r2][:, :], ["ysb%d" % r2], [("ys_d", j)])

    order = []
    for t_ in range(NWB * SST):
        j_ = (t_ % NWB) * SST + t_ // NWB
        if j_ < NSLT:
            order.append((j_, t_ % NWB))
    NB_ = len(order)
    for n_ in range(min(4, NB_)):
        fetchT(order[n_][0], n_ % 6)
    for n_ in range(min(2, NB_)):
        fetchX(order[n_][0], n_ % 3, n_ % 6)
    prev_user = {}
    lastpos = {}
    for n_ in range(NB_):
        prev_user[n_] = lastpos.get(order[n_][1])
        lastpos[order[n_][1]] = n_
    next_w = 0
    for n_ in range(NB_):
        if n_ + 4 < NB_:
            fetchT(order[n_ + 4][0], (n_ + 4) % 6)
        if n_ + 2 < NB_:
            fetchX(order[n_ + 2][0], (n_ + 2) % 3, (n_ + 2) % 6)
        while next_w < NB_ and next_w <= n_ + NWB - 1 and (prev_user[next_w] is None or prev_user[next_w] < n_):
            fetchW(order[next_w][0], order[next_w][1])
            next_w += 1
        assert next_w > n_
        tileB(order[n_][0], order[n_][1], n_ % 3, n_ % 2)

    io32b = cfc("IO32", 32).unsqueeze(1).to_broadcast([128, 32, 32])
    for c0 in range(0, 2 * NT, 32):
        n_ = min(32, 2 * NT - c0)
        bv = big[:, 0:n_ * 32].rearrange("p (c e) -> p c e", e=32)
        tt("dve", bv, cfc("IO32", 32).unsqueeze(1).to_broadcast([128, n_, 32]), EG[:, c0:c0 + n_].unsqueeze(2).to_broadcast([128, n_, 32]),
           ALU.is_equal, ["cf", "EG"], ["big"])
        tt("dve", bv, bv, pst[:, :].unsqueeze(1).to_broadcast([128, n_, 32]), ALU.mult, ["big", "pst"], ["big"])
        P.add("dve", lambda e, c0=c0, n_=n_, bv=bv: e.tensor_reduce(out=PB[:, c0:c0 + n_], in_=bv, axis=AX.X, op=ALU.add),
              reads=["big"], writes=["PB"])
    tt("dve", PB[:, :], PB[:, :], RK[:, :], ALU.add, ["PB", "RK"], ["PB"])
    cp("dve", POSI[:, :], PB[:, :], ["PB"], ["POSI"])

    def fetchC(i):
        r2 = i % 3
        P.add("pool", lambda e: e.indirect_dma_start(out=y0[r2][:, :], out_offset=None, in_=ys_d.ap(),
                                                     in_offset=bass.IndirectOffsetOnAxis(ap=POSI[:, 2 * i:2 * i + 1], axis=0)),
              reads=["POSI"] + YS_KEYS, writes=["y0_%d" % r2], dma=True, cost=1.0, dmat=1.05)
        P.add("pool", lambda e: e.indirect_dma_start(out=y1[r2][:, :], out_offset=None, in_=ys_d.ap(),
                                                     in_offset=bass.IndirectOffsetOnAxis(ap=POSI[:, 2 * i + 1:2 * i + 2], axis=0)),
              reads=["POSI"] + YS_KEYS, writes=["y1_%d" % r2], dma=True, cost=1.0, dmat=1.05)
        dma("sp", x1c[r2][:, :], x1_d.ap()[i * 128:(i + 1) * 128, :], [("x1_d", i)], ["x1c%d" % r2])

    def tileC(i):
        r2 = i % 3
        act(yc[:, :], y0[r2][:, :], AF.Identity, ["y0_%d" % r2, "WT"], ["yc"], scale=WT[:, 2 * i:2 * i + 1])
        stt(yc[:, :], y1[r2][:, :], WT[:, 2 * i + 1:2 * i + 2], yc[:, :], ALU.mult, ALU.add, ["y1_%d" % r2, "WT", "yc"], ["yc"])
        act(junkC[:, :], yc[:, :], AF.Square, ["yc"], ["junkC", "ssy"], accum=smC[:, 0:1])
        rstd(smC[:, 0:1], 1.0 / 1024, smC[:, 2:3], smC[:, 1:2], "ssy", "lny", "rsy")
        stt(tC[:, :], yc[:, :], smC[:, 2:3], G2b[:, :], ALU.mult, ALU.mult, ["yc", "rsy", "G2b"], ["tC"])
        tt("pool", x1c[r2][:, 0:512], tC[:, 0:512], x1c[r2][:, 0:512], ALU.add, ["tC", "x1c%d" % r2], [("x1o", r2, 0)])
        tt("dve", x1c[r2][:, 512:1024], tC[:, 512:1024], x1c[r2][:, 512:1024], ALU.add, ["tC", "x1c%d" % r2], [("x1o", r2, 1)])
        dma("sp", out_d.ap()[i * 128:(i + 1) * 128, :], x1c[r2][:, :], [("x1o", r2, 0), ("x1o", r2, 1)], [("out_d", i), "x1c%d" % r2])

    fetchC(0)
    if NT > 1:
        fetchC(1)
    for i in range(NT):
        if i + 2 < NT:
            fetchC(i + 2)
        tileC(i)
    P.add("sp", None, reads=[("out_d", i) for i in range(NT)] + [("x1_d", i) for i in range(NT)] + H2_KEYS + MOD_KEYS + ([("cat_d", i) for i in range(NT)] if debug else []))
    if SCHED:
        P.schedule()
    if _os.environ.get("NO_EMIT") == "1":
        return None
    stats = P.emit(nc, st)
    print("ops per engine, waits:", stats)
    st.close()
    return nc


def permute_w_in(w_in):
    q = w_in[:, 0:512].reshape(1024, 2, 4, 64).transpose(0, 2, 1, 3).reshape(1024, 512)
    return np.ascontiguousarray(np.concatenate([q, w_in[:, 512:]], axis=1))


def make_in_maps(NT, nb, inputs):
    f32 = np.float32
    cf, cb, ci = make_consts(NT)
    T = NT * 128
    shared = {
        "w_ada": np.ascontiguousarray(inputs["w_ada"][0], f32),
        "b_ada": np.ascontiguousarray(inputs["b_ada"][0:1], f32),
        "w_in": permute_w_in(np.asarray(inputs["w_in"][0], f32)),
        "w_out": np.ascontiguousarray(inputs["w_out"][0], f32),
        "w_r": np.ascontiguousarray(np.concatenate([inputs["w_router_group"][0], inputs["w_router_expert"][0]], axis=1), f32),
        "b_r": np.ascontiguousarray(np.concatenate([inputs["b_router_group"][0], inputs["b_router_expert"][0]])[None, :], f32),
        "ln1_pre": np.ascontiguousarray(inputs["ln1_pre"][0:1], f32),
        "ln1_post": np.ascontiguousarray(inputs["ln1_post"][0:1], f32),
        "ln2_pre": np.ascontiguousarray(inputs["ln2_pre"][0:1], f32),
        "ln2_post": np.ascontiguousarray(inputs["ln2_post"][0:1], f32),
        "sinks": np.ascontiguousarray(inputs["attn_sinks"][0:1], f32),
        "attn_norm": np.ascontiguousarray(inputs["attn_out_norm"][0:1], f32),
        "hgrn_norm": np.ascontiguousarray(inputs["hgrn_out_norm"][0:1], f32),
        "lbc": np.ascontiguousarray(np.asarray(inputs["hgrn_lb"], f32).reshape(2, 4, 128).transpose(2, 0, 1).reshape(128, 8)),
        "w_gate": np.ascontiguousarray(inputs["w_exp_gate"][0], f32),
        "w_up": np.ascontiguousarray(inputs["w_exp_up"][0], f32),
        "w_down": np.ascontiguousarray(inputs["w_exp_down"][0], f32),
        "cf": cf, "cb": cb, "ci": ci,
    }
    maps = []
    for b in range(nb):
        m = dict(shared)
        m["x"] = np.ascontiguousarray(inputs["x"][b, :T], f32)
        m["ccol"] = np.ascontiguousarray(np.asarray(inputs["c"][b], f32).reshape(8, 128).T)
        maps.append(m)
    return maps


_NC_CACHE = {}


def kernel(**inputs):
    NT = 64
    if NT not in _NC_CACHE:
        _NC_CACHE[NT] = build(NT)
    nc = _NC_CACHE[NT]
    maps = make_in_maps(NT, 8, inputs)
    res = run_bass_kernel_spmd(nc, maps, core_ids=list(range(8)))
    out = np.stack([np.asarray(r["out"], np.float32) for r in res.results], axis=0)
    return out
```

```python
import os as _os
import numpy as np
import ml_dtypes
from contextlib import ExitStack
import concourse.bass as bass
import concourse.mybir as mybir
from concourse.bass_utils import run_bass_kernel_spmd
from concourse.alu_op_type import AluOpType as ALU

AF = mybir.ActivationFunctionType
AX = mybir.AxisListType
F32 = mybir.dt.float32
BF16 = mybir.dt.bfloat16
I32 = mybir.dt.int32
U32 = mybir.dt.uint32

NDSEM = 40
NWB = int(_os.environ.get('NWB', '4'))
SCHED = _os.environ.get("NOSCHED") != "1"
SCHED_SEGS = _os.environ.get('SCHED_SEGS', 'all')
SBUF_BASE = 16512
SBUF_LIMIT = 229344


def dtsize(dt):
    return 2 if dt == BF16 else 4


class Op:
    __slots__ = ("eng", "fn", "dma", "deps", "alldeps", "inc", "cnt", "dsem", "dval", "dprev", "cost", "dmat", "barwait", "idx", "psrd", "pstag", "t0", "t1", "tset")


PRI_MODE = _os.environ.get('PRI_MODE', 'cp')
PRI_CP_SEGS = _os.environ.get('PRI_CP_SEGS', 'all')
PRI_K = float(_os.environ.get('PRI_K', '0.05'))
TBL_AWARE = _os.environ.get('TBL_AWARE', '0') == '1'
TBL_WINDOW = int(_os.environ.get('TBL_WINDOW', '200'))
SYNC_LAT = float(_os.environ.get('SYNC_LAT', '0.15'))
DMA_LAT = 2.0


PS_CUR = {}
BC_REG = [None]


class Prog:
    def __init__(self):
        self.ops = []
        self.res = {}
        self.segs = [0]
        self.fixed = set()

    def add(self, eng, fn, reads=(), writes=(), dma=False, cost=0.1, dmat=0.0, tset=None):
        op = Op()
        op.tset = tset
        op.eng, op.fn, op.dma, op.inc, op.cnt = eng, fn, dma, False, 0
        op.cost, op.dmat, op.barwait = cost, dmat, False
        op.psrd = any(isinstance(r, str) and r.startswith("ps") for r in reads)
        op.pstag = None
        for r in list(reads) + list(writes):
            if isinstance(r, str) and r.startswith("ps") and r in PS_CUR:
                op.pstag = PS_CUR[r]
        _nw = _os.environ.get("ANALYZE_NOWAR_KEYS")

        def relaxed(key):
            if not _nw:
                return False
            wk = key if isinstance(key, str) else str(key[0])
            if _nw == "ALL":
                return True
            if _nw == "SBUF":
                return not wk.startswith("ps")
            return any(wk.startswith(p) for p in _nw.split(","))
        deps = []
        for r in reads:
            st = self.res.get(r)
            if st is None:
                st = self.res[r] = [None, {}, []]
            if st[0] is not None:
                deps.append(st[0])
            if isinstance(r, str) and r.startswith("ps") and not relaxed(r):
                for lst in st[1].values():
                    deps.extend(lst)
        for w in writes:
            st = self.res.get(w)
            if st is None:
                st = self.res[w] = [None, {}, []]
            if relaxed(w):
                continue
            if st[0] is not None:
                deps.append(st[0])
            for lst in st[1].values():
                deps.extend(lst)
            deps.extend(st[2])
        for r in reads:
            st = self.res[r]
            if dma:
                st[2].append(op)
            else:
                st[1].setdefault(eng, []).append(op)
        for w in writes:
            st = self.res[w]
            st[0] = op
            st[1] = {}
            st[2] = []
        seen = set()
        d2 = []
        dall = []
        for d in deps:
            if id(d) in seen or d is op:
                continue
            seen.add(id(d))
            dall.append(d)
            if (not d.dma) and (not dma) and d.eng == eng and eng == "pe":
                continue
            d2.append(d)
        op.deps = d2
        op.alldeps = dall
        self.ops.append(op)
        return op

    def barrier(self, markers):
        ms = []
        self.segs.append(len(self.ops))
        self.fixed.add(len(self.segs) - 1)
        for n, (eng, fn) in enumerate(markers):
            m = self.add(eng, fn, writes=[("bar", len(self.ops), n)])
            m.inc = True
            ms.append(m)
        for eng in ["pe", "act", "pool", "dve", "sp"]:
            b = self.add(eng, None)
            b.deps = list(ms)
            b.alldeps = list(ms)
            b.barwait = True
        self.segs.append(len(self.ops))

    def schedule(self):
        import heapq
        bounds = self.segs + [len(self.ops)]
        new_ops = []
        for si in range(len(bounds) - 1):
            ops = self.ops[bounds[si]:bounds[si + 1]]
            n = len(ops)
            if n == 0:
                continue
            if si in self.fixed or (SCHED_SEGS != 'all' and str(si) not in SCHED_SEGS.split(',')):
                new_ops.extend(ops)
                continue
            pos = {id(op): k for k, op in enumerate(ops)}
            succ = [[] for _ in range(n)]
            npred = [0] * n
            chain = _os.environ.get('SCHED_CHAIN', '').split(',')
            lastk = {}
            for k, op in enumerate(ops):
                for d in op.alldeps:
                    j = pos.get(id(d))
                    if j is not None:
                        succ[j].append(k)
                        npred[k] += 1
                if op.eng in chain:
                    if op.eng in lastk:
                        succ[lastk[op.eng]].append(k)
                        npred[k] += 1
                    lastk[op.eng] = k
            ready = {e: [] for e in ["pe", "act", "pool", "dve", "sp"]}
            rtime = [0.0] * n
            PSB_ = int(_os.environ.get("PS_BONUS", "0"))
            pri = [k - (PSB_ if ops[k].psrd else 0) for k in range(n)]
            if PRI_MODE == "cp" and (PRI_CP_SEGS == "all" or str(si) in PRI_CP_SEGS.split(",")):
                bl = [0.0] * n
                for k in range(n - 1, -1, -1):
                    m_ = 0.0
                    for q_ in succ[k]:
                        if bl[q_] > m_:
                            m_ = bl[q_]
                    c_ = ops[k].cost + (ops[k].dmat + DMA_LAT if ops[k].dma else 0.0)
                    bl[k] = c_ + SYNC_LAT + m_
                w_ = float(_os.environ.get("PRI_W", "1.0"))
                pri = [k * PRI_K - w_ * bl[k] for k in range(n)]
            for k in range(n):
                if npred[k] == 0:
                    heapq.heappush(ready[ops[k].eng], k)
            eng_free = {e: 0.0 for e in ready}
            cur_tset = [None]
            nloads = 0
            start = [0.0] * n
            events = []
            dma_free = 0.0
            t = 0.0
            done = 0
            while done < n:
                while events and events[0][0] <= t + 1e-9:
                    ft, k = heapq.heappop(events)
                    done += 1
                    for m in succ[k]:
                        npred[m] -= 1
                        rt = ft + (0.0 if (ops[m].eng == ops[k].eng and ops[k].eng == "pe") else SYNC_LAT)
                        if rt > rtime[m]:
                            rtime[m] = rt
                        if npred[m] == 0:
                            heapq.heappush(ready[ops[m].eng], m)
                for e in ready:
                    if eng_free[e] > t + 1e-9 or not ready[e]:
                        continue
                    cand = [k for k in ready[e] if rtime[k] <= t + 1e-9]
                    if not cand:
                        continue
                    if e == "act":
                        oldest = min(cand)
                        same = [c for c in cand if ops[c].tset is None or ops[c].tset == cur_tset[0]]
                        if TBL_AWARE and same and (min(same) - oldest) < TBL_WINDOW:
                            k = min(same)
                        else:
                            k = min(cand, key=lambda c: (pri[c], c))
                        if ops[k].tset is not None and ops[k].tset != cur_tset[0]:
                            cur_tset[0] = ops[k].tset
                            t_extra = 1.3 if TBL_AWARE else 0.0
                            nloads += 1
                        else:
                            t_extra = 0.0
                    else:
                        k = min(cand, key=lambda c: (pri[c], c))
                        t_extra = 0.0
                    ready[e].remove(k)
                    heapq.heapify(ready[e])
                    op = ops[k]
                    start[k] = t
                    op.t0 = t
                    eng_free[e] = t + op.cost + t_extra
                    if op.dma:
                        b = max(dma_free, t + op.cost)
                        dma_free = b + op.dmat
                        fin = dma_free + DMA_LAT
                    else:
                        fin = t + op.cost + t_extra
                    op.t1 = fin
                    heapq.heappush(events, (fin, k))
                nxt = []
                if events:
                    nxt.append(events[0][0])
                for e in ready:
                    if ready[e]:
                        tm = max(eng_free[e], min(rtime[k] for k in ready[e]))
                        nxt.append(tm)
                if not nxt:
                    break
                t2 = min(nxt)
                t = t2 if t2 > t else t + 1e-6
            if _os.environ.get("SIM_DBG") == "1" and si == 0:
                firsts = {}
                for k in range(n):
                    op = ops[k]
                    key = (op.eng, round(op.cost, 3), op.dma)
                    if key not in firsts:
                        firsts[key] = (start[k], k)
                for key, v in sorted(firsts.items(), key=lambda kv: kv[1][0])[:40]:
                    print("     first", key, "t0=%.1f idx=%d" % v)
            order = sorted(range(n), key=lambda k: (start[k], k))
            new_ops.extend(ops[k] for k in order)
            tot = {}
            for op in ops:
                tot[op.eng] = tot.get(op.eng, 0.0) + op.cost
            if _os.environ.get("PS_STATS") == "1":
                iv = {}
                for op in ops:
                    if op.pstag is not None:
                        a_, b_ = iv.get(op.pstag, (1e18, 0.0))
                        iv[op.pstag] = (min(a_, op.t0), max(b_, op.t1))
                agg = {}
                for (ser, tag), (a_, b_) in iv.items():
                    agg.setdefault(tag, []).append(b_ - a_)
                for tag, v in sorted(agg.items(), key=lambda kv: -sum(kv[1])):
                    print("    ps hold %-10s n=%4d mean %.2f us total %.0f" % (tag, len(v), sum(v) / len(v), sum(v)))
            print("  segment %d: %d ops, tblloads %d, simulated makespan %.1f us; busy per engine: %s; dma busy %.1f" % (
                si, n, nloads, max(eng_free.values()), {e: round(v, 1) for e, v in tot.items()}, sum(op.dmat for op in ops if op.dma)))
        self.ops = new_ops

    def emit(self, nc, stack):
        engs = ["pe", "act", "pool", "dve", "sp"]
        csem = {e: stack.enter_context(nc.semaphore("c_" + e)) for e in engs}
        dsem = [stack.enter_context(nc.semaphore("d_%d" % i)) for i in range(NDSEM)]
        for i_, op in enumerate(self.ops):
            op.idx = i_
        for op in self.ops:
            best = {}
            nd = []
            for d in op.deps:
                if d.dma:
                    nd.append(d)
                else:
                    b_ = best.get(d.eng)
                    if b_ is None or d.idx > b_.idx:
                        best[d.eng] = d
            for d in best.values():
                assert d.idx < op.idx
                d.inc = True
                nd.append(d)
            op.deps = nd
        cnt = {e: 0 for e in engs}
        uses = [0] * NDSEM
        ndma = 0
        snap = {}
        for op in self.ops:
            if op.dma:
                slot = ndma % NDSEM
                ndma += 1
                op.dsem = slot
                op.dprev = 16 * uses[slot]
                uses[slot] += 1
                op.dval = 16 * uses[slot]
            elif op.inc:
                cnt[op.eng] += 1
                op.cnt = cnt[op.eng]
            if op.barwait:
                snap[id(op)] = list(uses)
        per = {e: [] for e in engs}
        for op in self.ops:
            per[op.eng].append(op)
        nwait = [0]

        def run(e, name):
            known = {}
            if name == "pool":
                BC_REG[0] = e.alloc_register(name="bcreg")
                e.reg_mov(BC_REG[0], 4095)
            for op in per[name]:
                waits = {}
                for d in op.deps:
                    if d.dma:
                        key, val = ("d", d.dsem), d.dval
                    else:
                        key, val = ("c", d.eng), d.cnt
                    if known.get(key, 0) >= val:
                        continue
                    if waits.get(key, 0) < val:
                        waits[key] = val
                if op.dma and op.dprev > 0:
                    key = ("d", op.dsem)
                    if known.get(key, 0) < op.dprev and waits.get(key, 0) < op.dprev:
                        waits[key] = op.dprev
                if op.barwait:
                    for slot, u in enumerate(snap[id(op)]):
                        key = ("d", slot)
                        if u > 0 and known.get(key, 0) < 16 * u:
                            waits[key] = 16 * u
                for key, val in waits.items():
                    s = dsem[key[1]] if key[0] == "d" else csem[key[1]]
                    e.wait_ge(s, val)
                    known[key] = val
                    nwait[0] += 1
                if op.fn is None:
                    continue
                ins = op.fn(e)
                if op.dma:
                    ins.then_inc(dsem[op.dsem], 16)
                elif op.inc:
                    ins.then_inc(csem[name], 1)

        with nc.Block() as block:
            @block.tensor
            def _(e):
                run(e, "pe")

            @block.scalar
            def _(e):
                run(e, "act")

            @block.gpsimd
            def _(e):
                run(e, "pool")

            @block.vector
            def _(e):
                run(e, "dve")

            @block.sync
            def _(e):
                run(e, "sp")
        return {e: len(per[e]) for e in engs}, nwait[0]


class Arena:
    def __init__(self, nc, base):
        self.nc, self.top = nc, base
        self.offs = {}

    def t(self, name, shape, dt):
        nbytes = int(np.prod(shape[1:])) * dtsize(dt)
        off = (self.top + 31) // 32 * 32
        self.top = off + nbytes
        assert self.top <= SBUF_LIMIT, (name, self.top)
        self.offs[name] = off
        return self.nc.alloc_sbuf_tensor_at(name, list(shape), dt, offset=off)

    def at(self, name, shape, dt, base_name, extra=0):
        return self.nc.alloc_sbuf_tensor_at(name, list(shape), dt, offset=self.offs[base_name] + extra)


def cf_layout(NT):
    NSLT = 2 * NT + 32
    L = {}
    o = 0
    for name, n in [("E", 2048), ("RM", 512), ("MJ", 4), ("IO32", 32), ("PIDX", 1), ("EPS", 1),
                    ("ONE", 1), ("NEG", 4), ("MG", NT), ("JG", NSLT)]:
        L[name] = o
        o += n
    L["N"] = o
    return L


def make_consts(NT):
    NSLT = 2 * NT + 32
    L = cf_layout(NT)
    cf = np.zeros((128, L["N"]), np.float32)
    k = np.arange(128)[:, None]
    q = np.arange(128)[None, :]
    for kb in range(2):
        for g in range(2):
            for r in range(4):
                h = 4 * g + r
                slope = 2.0 ** (-(h + 1))
                if kb == 1:
                    dist = q - k
                    valid = k <= q
                else:
                    dist = q + 128 - k
                    valid = k > q
                e = np.where(valid, np.exp(-slope * dist.astype(np.float64)), 0.0)
                c0 = L["E"] + (kb * 2 + g) * 512 + r * 128
                cf[:, c0:c0 + 128] = e
    rm = np.ones(512, np.float32)
    rm[::32] = 0.0
    cf[:, L["RM"]:L["RM"] + 512] = rm[None, :]
    for j in range(4):
        cf[32 * j:32 * j + 32, L["MJ"] + j] = 1.0
    cf[:, L["IO32"]:L["IO32"] + 32] = np.arange(32)[None, :]
    cf[:, L["PIDX"]] = np.arange(128)
    cf[:, L["EPS"]] = 1e-6
    cf[:, L["ONE"]] = 1.0
    cf[:, L["NEG"]:L["NEG"] + 4] = -1e30
    cf[:, L["MG"]:L["MG"] + NT] = 128.0 * np.arange(NT)[None, :]
    cf[:, L["JG"]:L["JG"] + NSLT] = np.arange(NSLT)[None, :]
    cb = np.zeros((128, 512), np.float32)
    cb[:, 0:128] = np.eye(128)
    s = np.arange(128)[:, None]
    c = np.arange(128)[None, :]
    cb[:, 128:256] = ((s // 32 == c // 32) & (s <= c)).astype(np.float32)
    cb[:, 256:384] = (s < c).astype(np.float32)
    cb[:, 384:512] = 1.0
    ci = (np.arange(NT)[None, :] * 128 + np.arange(128)[:, None]).astype(np.int32)
    return cf, cb.astype(ml_dtypes.bfloat16), ci


def build(NT, debug=False):
    T = NT * 128
    NSLT = 2 * NT + 32
    CAP = T
    NINV = 32 * CAP
    SST = (NSLT + NWB - 1) // NWB
    L = cf_layout(NT)
    nc = bass.Bass("TRN2", target_bir_lowering=False)
    P = Prog()

    def din(name, shape, dt):
        return nc.dram_tensor(name, list(shape), dt, kind="ExternalInput")

    def dscr(name, shape, dt, out=False):
        return nc.dram_tensor(name, list(shape), dt, kind="ExternalOutput" if out else "Internal")

    x_d = din("x", [T, 1024], F32)
    ccol_d = din("ccol", [128, 8], F32)
    wada_d = din("w_ada", [1024, 6144], F32)
    bada_d = din("b_ada", [1, 6144], F32)
    win_d = din("w_in", [1024, 2816], F32)
    wout_d = din("w_out", [1024, 1024], F32)
    wr_d = din("w_r", [1024, 36], F32)
    br_d = din("b_r", [1, 36], F32)
    ln1pre_d = din("ln1_pre", [1, 1024], F32)
    ln1post_d = din("ln1_post", [1, 1024], F32)
    ln2pre_d = din("ln2_pre", [1, 1024], F32)
    ln2post_d = din("ln2_post", [1, 1024], F32)
    sinks_d = din("sinks", [1, 8], F32)
    an_d = din("attn_norm", [1, 512], F32)
    hn_d = din("hgrn_norm", [1, 512], F32)
    lbc_d = din("lbc", [128, 8], F32)
    wg_d = din("w_gate", [32, 1024, 256], F32)
    wu_d = din("w_up", [32, 1024, 256], F32)
    wd_d = din("w_down", [32, 256, 1024], F32)
    cf_d = din("cf", [128, L["N"]], F32)
    cb_d = din("cb", [128, 512], BF16)
    ci_d = din("ci", [128, NT], I32)
    out_d = nc.dram_tensor("out", [T, 1024], F32, kind="ExternalOutput")
    mod_d = dscr("mod_s", [1, 6144], F32, out=debug)
    x1_d = dscr("x1_s", [T, 1024], F32, out=debug)
    h2_d = dscr("h2_s", [T, 1024], BF16, out=debug)
    inv_d = dscr("inv_s", [NINV, 1], I32)
    wall_d = dscr("wall_s", [4096, 6144], BF16)
    ys_d = dscr("ys_s", [NSLT * 128, 1024], BF16)
    cat_d = dscr("cat_s", [T, 1024], BF16, out=True) if debug else None

    st = ExitStack()
    pers = Arena(nc, SBUF_BASE)
    cf = pers.t("cf", [128, L["N"]], F32)
    cb = pers.t("cb", [128, 512], BF16)
    ci = pers.t("ci", [128, NT], I32)
    G2b = pers.t("G2b", [128, 1024], F32)
    EG = pers.t("EG", [128, 2 * NT], F32)
    WT = pers.t("WT", [128, 2 * NT], F32)
    RK = pers.t("RK", [128, 2 * NT], F32)
    POSI = pers.t("POSI", [128, 2 * NT], I32)
    Cb = pers.t("Cb", [128, 32], F32)
    IDXJ = pers.t("IDXJ", [128, NSLT], I32)
    WIDX = pers.t("WIDX", [128, NSLT], I32)
    dmy = pers.t("dmy", [128, 24], F32)
    ident = cb[:, 0:128]
    CM = cb[:, 128:256]
    Lst = cb[:, 256:384]
    ones_bf = cb[:, 384:512]

    def cfc(name, n=1, off=0):
        return cf[:, L[name] + off:L[name] + off + n]

    A = Arena(nc, pers.top)
    w_in_bf = A.t("w_in_bf", [128, 8, 2816], BF16)
    w_out_bf = A.t("w_out_bf", [128, 8, 1024], BF16)
    w_r_bf = A.t("w_r_bf", [128, 8, 36], BF16)
    GS1b = A.t("GS1b", [128, 1024], F32)
    SH1b = A.t("SH1b", [128, 1024], F32)
    G1b = A.t("G1b", [128, 1024], F32)
    GS2b = A.t("GS2b", [128, 1024], F32)
    SH2b = A.t("SH2b", [128, 1024], F32)
    ANb = A.t("ANb", [128, 512], F32)
    HNb = A.t("HNb", [128, 512], F32)
    BRb = A.t("BRb", [128, 36], F32)
    exsink = A.t("exsink", [128, 8], F32)
    lbc = A.t("lbc", [128, 8], F32)
    oml = A.t("oml", [128, 4], F32)
    homl = A.t("homl", [128, 4], F32)
    bhoml = A.t("bhoml", [128, 4], F32)
    XT = [A.t("xt%d" % i, [128, 1024], F32) for i in range(3)]
    tA = A.t("tA", [128, 1024], F32)
    tA2 = A.t("tA2", [128, 1024], F32)
    tA3 = A.t("tA3", [128, 1024], F32)
    junk = A.t("junk", [128, 1024], BF16)
    Hb = A.t("Hb", [128, 1024], BF16)
    CATb = A.t("CATb", [128, 1024], BF16)
    H2b = A.t("H2b", [128, 1024], BF16)
    hT = A.t("hT", [128, 8, 128], BF16)
    catT = A.t("catT", [128, 8, 128], BF16)
    h2T = A.t("h2T", [128, 8, 128], BF16)
    stage = [A.t("stage%d" % i, [128, 2048], BF16) for i in range(2)]
    wcv = [A.t("wcv%d" % i, [128, 2048], BF16) for i in range(2)]
    qT_sb = A.t("qT_sb", [128, 4, 128], BF16)
    kT = [A.t("kT%d" % i, [128, 128], BF16) for i in range(2)]
    vaug = [A.t("vaug%d" % i, [128, 2, 65], BF16) for i in range(2)]
    pe_sb = [A.t("pe_sb%d" % i, [128, 512], BF16) for i in range(4)]
    pT = [A.t("pT%d" % i, [128, 512], BF16) for i in range(4)]
    attn_o = A.t("attn_o", [128, 512], F32)
    qs = A.t("qs", [128, 512], F32)
    th = A.t("th", [128, 512], F32)
    sg = A.t("sg", [128, 512], F32)
    logf = A.t("logf", [128, 512], F32)
    bcs = A.t("bcs", [128, 512], F32)
    enb = A.t("enb", [128, 512], F32)
    dec = A.t("dec", [128, 16], F32)
    kdT = A.t("kdT", [128, 4, 128], BF16)
    qdT = A.t("qdT", [128, 4, 128], BF16)
    keT = A.t("keT", [128, 4, 128], BF16)
    ke_tm = A.t("ke_tm", [128, 4, 128], BF16)
    Vz = A.t("Vz", [128, 4, 512], BF16)
    v_bf = A.t("v_bf", [128, 512], BF16)
    gsn = A.t("gsn", [128, 512], F32)
    osb = A.t("osb", [128, 512], F32)
    zt = A.at("zt", [128, 512], I32, "osb")
    aTm = A.t("aTm", [128, 4, 128], BF16)
    Sb = A.t("Sb", [128, 4, 4, 128], BF16)
    Sprev = A.t("Sprev", [128, 4, 128], BF16)
    smA = A.t("smA", [128, 64], F32)
    smR = A.t("smR", [128, 160], F32)
    smU = A.t("smU", [128, 16], U32)
    smI = A.t("smI", [128, 2], I32)
    O01 = A.t("O01", [128, 64], BF16)
    modc = [A.t("modc%d" % i, [1, 256], F32) for i in range(2)]
    badac = [A.t("badac%d" % i, [1, 256], F32) for i in range(2)]
    ccol = A.t("ccol_s", [128, 8], F32)
    cact = A.t("cact", [128, 8], BF16)
    a_top = A.top

    B = Arena(nc, pers.top)
    wall = [B.t("wall%d" % i, [128, 6144], BF16) for i in range(NWB)]
    tokidx = [B.t("tokidx%d" % i, [128, 1], I32) for i in range(6)]
    xg = [B.t("xg%d" % i, [128, 1024], BF16) for i in range(3)]
    xgT = [B.t("xgT%d" % i, [128, 8, 128], BF16) for i in range(2)]
    sgm = B.t("sgm", [128, 256], F32)
    actT = [B.t("actT%d" % i, [128, 2, 128], BF16) for i in range(2)]
    ysb = [B.t("ysb%d" % i, [128, 1024], BF16) for i in range(2)]
    y0 = [B.t("y0_%d" % i, [128, 1024], BF16) for i in range(3)]
    y1 = [B.t("y1_%d" % i, [128, 1024], BF16) for i in range(3)]
    x1c = [B.t("x1c%d" % i, [128, 1024], F32) for i in range(3)]
    yc = B.t("yc", [128, 1024], F32)
    tC = B.t("tC", [128, 1024], F32)
    junkC = B.t("junkC", [128, 1024], BF16)
    smC = B.t("smC", [128, 16], F32)
    big = B.t("big", [128, 1024], F32)
    tend = B.t("tend", [128, 32], F32)
    ntl = B.t("ntl", [128, 32], F32)
    tst = B.t("tst", [128, 32], F32)
    pst = B.t("pst", [128, 32], F32)
    PB = B.t("PB", [128, 2 * NT], F32)
    EXJ = B.t("EXJ", [128, NSLT], F32)
    TSJ = B.t("TSJ", [128, NSLT], F32)
    IJF = B.t("IJF", [128, NSLT], F32)
    RJ = B.t("RJ", [128, NSLT], F32)
    print("SBUF tops: pers %d A %d B %d" % (pers.top, a_top, B.top))

    PSB = [st.enter_context(nc.psum_tensor("psb%d" % i, [128, 512], F32)) for i in range(8)]
    PSG = {"E": [0], "SC": [1, 2], "PV": [3], "HG": [4, 5], "LT": [6, 7], "ALL": [0, 1, 2, 3, 4, 5, 6, 7]}
    psring = {g: 0 for g in PSG}
    pserial = [0]

    def psnew(grp, tag="?"):
        banks = PSG[grp]
        i_ = banks[psring[grp] % len(banks)]
        psring[grp] += 1
        pserial[0] += 1
        PS_CUR["ps%d" % i_] = (pserial[0], tag)
        return PSB[i_], "ps%d" % i_

    def fsz(ap):
        n = 1
        for d in list(ap.shape)[1:]:
            n *= int(d)
        return n

    def dma(eng, out, in_, reads, writes):
        nbytes = fsz(out) * int(out.shape[0]) * dtsize(out.dtype)
        return P.add(eng, lambda e: e.dma_start(out=out, in_=in_), reads=reads, writes=writes, dma=True,
                     cost=0.15, dmat=nbytes / 250e3)

    def idma(out, out_off, in_, in_off, reads, writes, nbytes):
        return P.add("pool", lambda e: e.indirect_dma_start(out=out, out_offset=out_off, in_=in_, in_offset=in_off),
                     reads=reads, writes=writes, dma=True, cost=1.0, dmat=nbytes / 250e3)

    def act(out, in_, func, reads, writes, scale=None, bias=None, accum=None):
        kw = {}
        if scale is not None:
            kw["scale"] = scale
        if bias is not None:
            kw["bias"] = bias
        if accum is not None:
            kw["accum_out"] = accum
        tset = 18 if func in (AF.Silu, AF.Tanh) else (6 if func in (AF.Exp, AF.Ln) else (2 if func == AF.Sigmoid else None))
        return P.add("act", lambda e: e.activation(out=out, in_=in_, func=func, **kw), reads=reads, writes=writes,
                     cost=0.25 + fsz(in_) * 0.0009, tset=tset)

    def ecost(eng, n):
        if eng == "pool":
            return 0.2 + n * 0.002
        if eng == "act":
            return 0.25 + n * 0.0009
        return 0.1 + n * 0.0015

    def tt(eng, out, in0, in1, op, reads, writes):
        return P.add(eng, lambda e: e.tensor_tensor(out=out, in0=in0, in1=in1, op=op), reads=reads, writes=writes,
                     cost=ecost(eng, fsz(out)))

    def ts(eng, out, in0, s1, op0, reads, writes, s2=None, op1=None):
        c = ecost(eng, fsz(out))
        if op1 is None:
            return P.add(eng, lambda e: e.tensor_scalar(out=out, in0=in0, scalar1=s1, scalar2=None, op0=op0), reads=reads, writes=writes, cost=c)
        return P.add(eng, lambda e: e.tensor_scalar(out=out, in0=in0, scalar1=s1, scalar2=s2, op0=op0, op1=op1), reads=reads, writes=writes, cost=c)

    def stt(out, in0, scalar, in1, op0, op1, reads, writes, accum=None):
        kw = {"accum_out": accum} if accum is not None else {}
        return P.add("dve", lambda e: e.scalar_tensor_tensor(out=out, in0=in0, scalar=scalar, in1=in1, op0=op0, op1=op1, **kw), reads=reads, writes=writes,
                     cost=ecost("dve", fsz(out)))

    def cp(eng, out, in_, reads, writes):
        c = ecost(eng, fsz(out))
        if eng == "act":
            return P.add("act", lambda e: e.copy(out=out, in_=in_), reads=reads, writes=writes, cost=c)
        return P.add(eng, lambda e: e.tensor_copy(out=out, in_=in_), reads=reads, writes=writes, cost=c)

    def mm(out, lhsT, rhs, start, stop, reads, writes, tp=None, sgc=False):
        kw = {"tile_position": tp} if tp is not None else {}
        if sgc:
            kw["skip_group_check"] = True
        c = 0.035 + fsz(rhs) * 0.00042 * (4 if rhs.dtype == F32 else 1)
        return P.add("pe", lambda e: e.matmul(out, lhsT=lhsT, rhs=rhs, start=start, stop=stop, **kw), reads=reads, writes=writes, cost=c)

    def rstd(ss_ap, inv_n, out_ap, tmp_ap, key_in, key_tmp, key_out):
        act(tmp_ap, ss_ap, AF.Ln, [key_in, "cf"], [key_tmp], scale=inv_n, bias=cfc("EPS")[:ss_ap.shape[0], :])
        act(out_ap, tmp_ap, AF.Exp, [key_tmp], [key_out], scale=-0.5)

    def transpose_tm(src, src_key, dst, dst_key, grp, nchunk=8):
        pb, pk = psnew(grp, "tr")
        pbv = pb[:, :].bitcast(BF16)
        for j in range(nchunk):
            P.add("pe", lambda e, j=j: e.transpose(out=pbv[:, j * 128:(j + 1) * 128], in_=src[:, j * 128:(j + 1) * 128], identity=ident),
                  reads=[src_key, "cb"], writes=[pk], cost=0.09)
        cp("act", dst.rearrange("p k t -> p (k t)")[:, 0:nchunk * 128], pbv[:, 0:nchunk * 128], [pk], [dst_key])

    dma("sp", cf[:, :], cf_d.ap(), [], ["cf"])
    dma("sp", cb[:, :], cb_d.ap(), [], ["cb"])
    dma("sp", ci[:, :], ci_d.ap(), [], ["ci"])
    dma("sp", ccol[:, :], ccol_d.ap(), [], ["ccol"])
    dma("sp", lbc[:, :], lbc_d.ap(), [], ["lbc"])
    dma("sp", BRb[:, :], br_d.ap()[0:1, :].partition_broadcast(128), [], ["BRb"])
    dma("sp", exsink[:, :], sinks_d.ap()[0:1, :].partition_broadcast(128), [], ["exsink"])
    dma("sp", ANb[:, :], an_d.ap()[0:1, :].partition_broadcast(128), [], ["ANb"])
    dma("sp", HNb[:, :], hn_d.ap()[0:1, :].partition_broadcast(128), [], ["HNb"])
    dma("sp", GS1b[:, :], ln1pre_d.ap()[0:1, :].partition_broadcast(128), [], ["GS1b"])
    dma("sp", G1b[:, :], ln1post_d.ap()[0:1, :].partition_broadcast(128), [], ["G1b"])
    dma("sp", GS2b[:, :], ln2pre_d.ap()[0:1, :].partition_broadcast(128), [], ["GS2b"])
    dma("sp", G2b[:, :], ln2post_d.ap()[0:1, :].partition_broadcast(128), [], ["G2b"])
    P.add("pool", lambda e: e.memset(zt[:, :], 0), writes=["osb"])
    P.add("pool", lambda e: e.memset(Cb[:, :], 0.0), writes=["Cb"])
    P.add("pool", lambda e: e.memset(Sprev[:, :, :], 0.0), writes=["Sprev"])
    P.add("pool", lambda e: e.memset(dmy[:, :], 0.0), writes=["dmy"])
    for i in range(2):
        P.add("pool", lambda e, i=i: e.memset(vaug[i][:, :, :], 1.0), writes=["vaug%d" % i])
    inv_v = inv_d.ap().rearrange("(p f) o -> p (f o)", p=128)
    ninv_f = NINV // 128
    INV0_KEYS = [("inv0", c0) for c0 in range(0, ninv_f, 512)]
    INV_KEYS = [("inv", i, k_) for i in range(NT) for k_ in range(2)]
    H2_KEYS = [("h2_d", i) for i in range(NT)]
    YS_KEYS = [("ys_d", j) for j in range(NSLT)]
    WGU_KEYS = [("wgu_d", e_, c) for e_ in range(32) for c in range(2)]
    WDN_KEYS = [("wdn_d", e_) for e_ in range(32)]
    for c0 in range(0, ninv_f, 512):
        c1 = min(c0 + 512, ninv_f)
        dma("sp", inv_v[:, c0:c1], zt[:, 0:c1 - c0], ["osb"], [("inv0", c0)])
    act(exsink[:, :], exsink[:, :], AF.Exp, ["exsink"], ["exsink"])
    tt("dve", oml[:, :], lbc[:, 4:8], lbc[:, 0:4], ALU.subtract, ["lbc"], ["oml"])
    act(oml[:, :], oml[:, :], AF.Sigmoid, ["oml"], ["oml"])
    ts("dve", homl[:, :], oml[:, :], 0.5, ALU.mult, ["oml"], ["homl"])
    ts("dve", bhoml[:, :], oml[:, :], -0.5, ALU.mult, ["oml"], ["bhoml"], s2=1.0, op1=ALU.add)

    sidx = [0]

    def stage_load(src_ap, ncols_view):
        s = sidx[0] % 2
        sidx[0] += 1
        dma("sp", ncols_view(stage[s]), src_ap, [], ["stage%d" % s])
        return s

    def dmac(out, in_, reads, writes, nbytes):
        return P.add("pool", lambda e: e.dma_start(out=out, in_=in_), reads=reads, writes=writes, dma=True,
                     cost=1.0, dmat=nbytes / 250e3)
    CW = int(_os.environ.get("CAST_CW", "512"))
    WIN_KEYS, WOUT_KEYS = [], []
    for k in range(8):
        for c0 in range(0, 2816, CW):
            c1 = min(c0 + CW, 2816)
            dmac(w_in_bf[:, k, c0:c1], win_d.ap()[k * 128:(k + 1) * 128, c0:c1], [], [("w_in_c", k, c0)], 128 * (c1 - c0) * 4)
            WIN_KEYS.append(("w_in_c", k, c0))
    for k in range(8):
        for c0 in range(0, 1024, CW):
            c1 = min(c0 + CW, 1024)
            dmac(w_out_bf[:, k, c0:c1], wout_d.ap()[k * 128:(k + 1) * 128, c0:c1], [], [("w_out_c", k, c0)], 128 * (c1 - c0) * 4)
            WOUT_KEYS.append(("w_out_c", k, c0))
    dmac(w_r_bf[:, :, :], wr_d.ap().rearrange("(k p) n -> p k n", p=128), [], ["w_r_bf"], 128 * 288 * 4)
    P.add("pe", None, reads=WIN_KEYS, writes=["w_in_bf"], cost=0.01)
    P.add("pe", None, reads=WOUT_KEYS, writes=["w_out_bf"], cost=0.01)

    act(cact[:, :], ccol[:, :], AF.Silu, ["ccol"], ["cact"])
    wada_v = wada_d.ap().rearrange("(k p) n -> p k n", p=128)
    NCH = 24
    for n in range(NCH):
        s_ = n % 2
        sv = stage[s_][:, :].rearrange("p (k n) -> p k n", n=256)
        dmac(sv, wada_v[:, :, n * 256:(n + 1) * 256], [], ["stage%d" % s_], 1024 * 256 * 4)
        pb, pk = psnew("LT", "ada")
        for k in range(8):
            mm(pb[0:1, 0:256], cact[:, k:k + 1], sv[:, k, :], k == 0, k == 7, ["cact", "stage%d" % s_], [pk])
        r_ = n % 2
        dma("sp", badac[r_][0:1, :], bada_d.ap()[0:1, n * 256:(n + 1) * 256], [], ["badac%d" % r_])
        tt("dve", modc[r_][0:1, :], pb[0:1, 0:256], badac[r_][0:1, :], ALU.add, [pk, "badac%d" % r_], ["modc%d" % r_])
        dma("sp", mod_d.ap()[0:1, n * 256:(n + 1) * 256], modc[r_][0:1, :], ["modc%d" % r_], [("mod_d", n)])
    MOD_KEYS = [("mod_d", n) for n in range(NCH)]

    def modb(j):
        return mod_d.ap()[0:1, j * 1024:(j + 1) * 1024].partition_broadcast(128)

    def modk(j):
        return [("mod_d", n) for n in range(4 * j, 4 * j + 4)]
    dma("sp", SH1b[:, :], modb(0), modk(0), ["SH1b"])
    dma("sp", tA[:, :], modb(1), modk(1), ["tA"])
    stt(GS1b[:, :], tA[:, :], 1.0, GS1b[:, :], ALU.add, ALU.mult, ["tA", "GS1b"], ["GS1b"])
    dma("sp", XT[1][:, :], modb(2), modk(2), ["xt1"])
    tt("dve", G1b[:, :], XT[1][:, :], G1b[:, :], ALU.mult, ["xt1", "G1b"], ["G1b"])
    dma("sp", SH2b[:, :], modb(3), modk(3), ["SH2b"])
    dma("sp", XT[2][:, :], modb(4), modk(4), ["xt2"])
    stt(GS2b[:, :], XT[2][:, :], 1.0, GS2b[:, :], ALU.add, ALU.mult, ["xt2", "GS2b"], ["GS2b"])
    dma("sp", XT[1][:, :], modb(5), modk(5), ["xt1"])
    tt("dve", G2b[:, :], XT[1][:, :], G2b[:, :], ALU.mult, ["xt1", "G2b"], ["G2b"])

    wgu_dv = wall_d.ap()[:, 0:4096].rearrange("r (k n) -> r k n", n=512)
    wdn_dv = wall_d.ap()[:, 4096:6144].rearrange("r (f n) -> r f n", n=1024)
    jobs = []
    for e_ in range(32):
        jobs.append(("g", e_))
        jobs.append(("u", e_))
        jobs.append(("d", e_))
    wcvi = [0]

    wcvi = [0]
    CONV_GATE = [[]]

    def conv_job(job):
        kind, e_ = job
        w = wcvi[0] % 2
        wcvi[0] += 1
        if kind in ("g", "u"):
            src = (wg_d if kind == "g" else wu_d).ap()[e_].rearrange("(k p) n -> p k n", p=128)
            c0 = 0 if kind == "g" else 256
            dmac(wcv[w][:, :].rearrange("p (k n) -> p k n", n=256), src, CONV_GATE[0], ["wcv%d" % w], 1024 * 256 * 4)
            dma("sp", wgu_dv[e_ * 128:(e_ + 1) * 128, :, c0:c0 + 256], wcv[w][:, :].rearrange("p (k n) -> p k n", n=256),
                ["wcv%d" % w], [("wgu_d", e_, 0 if kind == "g" else 1)])
        else:
            src = wd_d.ap()[e_].rearrange("(f p) n -> p f n", p=128)
            dmac(wcv[w][:, :].rearrange("p (f n) -> p f n", n=1024), src, CONV_GATE[0], ["wcv%d" % w], 256 * 1024 * 4)
            dma("sp", wdn_dv[e_ * 128:(e_ + 1) * 128, :, :], wcv[w][:, :].rearrange("p (f n) -> p f n", n=1024),
                ["wcv%d" % w], [("wdn_d", e_)])

    def tileA(i):
        par = i % 2
        xt = XT[i % 3]
        xk = "xt%d" % (i % 3)
        rows = slice(i * 128, (i + 1) * 128)
        dma("sp", xt[:, :], x_d.ap()[rows, :], [], [xk])
        act(junk[:, :], xt[:, :], AF.Square, [xk], ["ss1"], accum=smA[:, 0:1])
        rstd(smA[:, 0:1], 1.0 / 1024, smA[:, 2:3], smA[:, 1:2], "ss1", "ln1", "rs1")
        stt(tA[:, :], xt[:, :], smA[:, 2:3], GS1b[:, :], ALU.mult, ALU.mult, [xk, "rs1", "GS1b"], ["tA"])
        tt("pool", Hb[:, :], tA[:, :], SH1b[:, :], ALU.add, ["tA", "SH1b"], ["Hb"])
        transpose_tm(Hb, "Hb", hT, "hT", "E")
        pq, kq = psnew("E", "q")
        for m in range(4):
            for k in range(8):
                mm(pq[:, m * 128:(m + 1) * 128], w_in_bf[:, k, m * 128:(m + 1) * 128], hT[:, k, :], k == 0, k == 7,
                   ["w_in_bf", "hT"], [kq])
        act(qT_sb.rearrange("p m t -> p (m t)"), pq[:, 0:512], AF.Identity, [kq], ["qT_sb"], scale=0.125)
        pkv, kkv = psnew("E", "kv")
        for k in range(8):
            mm(pkv[:, 0:128], w_in_bf[:, k, 512:640], hT[:, k, :], k == 0, k == 7, ["w_in_bf", "hT"], [kkv])
        for k in range(8):
            mm(pkv[:, 128:256], hT[:, k, :], w_in_bf[:, k, 640:768], k == 0, k == 7, ["w_in_bf", "hT"], [kkv])
        cp("dve", kT[par][:, :], pkv[:, 0:128], [kkv], ["kT%d" % par])
        cp("dve", vaug[par][:, :, 0:64], pkv[:, 128:256].rearrange("p (g d) -> p g d", d=64), [kkv], ["vaug%d" % par])
        for g in range(2):
            kbs = []
            if i > 0:
                kbs.append((1 - par, 0))
            kbs.append((par, 1))
            pvb, kvb = psnew("PV", "pv")
            for n, (bp, kb) in enumerate(kbs):
                ring = (2 * g + n) % 4
                psc, ksc = psnew("SC", "sc")
                mm(psc[:, :], kT[bp][64 * g:64 * g + 64, :], qT_sb[64 * g:64 * g + 64, :, :].rearrange("p m t -> p (m t)"),
                   True, True, ["kT%d" % bp, "qT_sb"], [ksc], tp=(64 * g, 0))
                act(pe_sb[ring][:, :], psc[:, :], AF.Exp, [ksc], ["pe_sb%d" % ring])
                c0 = L["E"] + (kb * 2 + g) * 512
                tt("pool", pT[ring][:, :], pe_sb[ring][:, :], cf[:, c0:c0 + 512], ALU.mult, ["pe_sb%d" % ring, "cf"], ["pT%d" % ring])
                for r in range(4):
                    mm(pvb[:, r * 65:(r + 1) * 65], pT[ring][:, r * 128:(r + 1) * 128], vaug[bp][:, g, :], n == 0 and r == 0,
                       n == len(kbs) - 1, ["pT%d" % ring, "vaug%d" % bp], [kvb], sgc=True)
            pv = pvb[:, 0:260].rearrange("p (r c) -> p r c", c=65)
            tt("dve", smA[:, 8:12], pv[:, :, 64], exsink[:, 4 * g:4 * g + 4], ALU.add, [kvb, "exsink"], ["den"])
            P.add("dve", lambda e: e.reciprocal(out=smA[:, 12:16], in_=smA[:, 8:12]), reads=["den"], writes=["rden"])
            tt("dve", attn_o[:, g * 256:(g + 1) * 256].rearrange("p (r d) -> p r d", d=64), pv[:, :, 0:64],
               smA[:, 12:16].unsqueeze(2).to_broadcast([128, 4, 64]), ALU.mult, [kvb, "rden"], ["attn_o"])
        act(junk[:, 0:512], attn_o[:, :], AF.Square, ["attn_o"], ["ssa"], accum=smA[:, 16:17])
        rstd(smA[:, 16:17], 1.0 / 512, smA[:, 18:19], smA[:, 17:18], "ssa", "lna", "rsa")
        stt(CATb[:, 0:512], attn_o[:, :], smA[:, 18:19], ANb[:, :], ALU.mult, ALU.mult, ["attn_o", "rsa", "ANb"], ["CATb"])
        phq, khq = psnew("HG", "hq")
        for h in range(4):
            for k in range(8):
                mm(phq[:, h * 128:(h + 1) * 128], w_in_bf[:, k, 768 + h * 128:768 + (h + 1) * 128], hT[:, k, :], k == 0, k == 7,
                   ["w_in_bf", "hT"], [khq])
        act(qs[:, :], phq[:, :], AF.Silu, [khq], ["qs"])
        phf, khf = psnew("HG", "hf")
        for h in range(4):
            for k in range(8):
                mm(phf[:, h * 128:(h + 1) * 128], w_in_bf[:, k, 1280 + h * 128:1280 + (h + 1) * 128], hT[:, k, :],
                   k == 0, k == 7, ["w_in_bf", "hT"], [khf])
        act(th[:, :], phf[:, :], AF.Tanh, [khf], ["th"], scale=0.5)
        phi, khi = psnew("HG", "hi")
        for k in range(8):
            mm(phi[:, :], hT[:, k, :], w_in_bf[:, k, 1792:2304], k == 0, k == 7, ["w_in_bf", "hT"], [khi])
        tt("dve", Vz[:, :, :], phi[:, :].unsqueeze(1).to_broadcast([128, 4, 512]),
           cfc("MJ", 4).unsqueeze(2).to_broadcast([128, 4, 512]), ALU.mult, [khi, "cf"], ["Vz"])
        cp("act", v_bf[:, :], phi[:, :], [khi], ["v_bf"])
        phg, khg = psnew("HG", "hg")
        for k in range(8):
            mm(phg[:, :], hT[:, k, :], w_in_bf[:, k, 2304:2816], k == 0, k == 7, ["w_in_bf", "hT"], [khg])
        act(gsn[:, :], phg[:, :], AF.Silu, [khg], ["gsn"])
        ts("pool", sg[:, :], th[:, :], -0.5, ALU.mult, ["th"], ["sg"], s2=0.5, op1=ALU.add)
        for h in range(4):
            act(logf[:, h * 128:(h + 1) * 128], th[:, h * 128:(h + 1) * 128], AF.Ln, ["th", "homl", "bhoml"], ["logf"],
                scale=homl[:, h:h + 1], bias=bhoml[:, h:h + 1])
        P.add("dve", lambda e: e.tensor_tensor_scan(out=bcs[:, :], data0=cfc("RM", 512), data1=logf[:, :], initial=0.0,
                                                    op0=ALU.mult, op1=ALU.add), reads=["logf", "cf"], writes=["bcs"], cost=1.15)
        act(logf[:, :], bcs[:, :], AF.Exp, ["bcs"], ["logf"])
        act(enb[:, :], bcs[:, :], AF.Exp, ["bcs"], ["enb"], scale=-1.0)
        act(dec[:, :], bcs[:, :].rearrange("p (c t) -> p c t", t=32)[:, :, 31], AF.Exp, ["bcs"], ["dec"])
        for h in range(4):
            stt(kdT[:, h, :], sg[:, h * 128:(h + 1) * 128], oml[:, h:h + 1], enb[:, h * 128:(h + 1) * 128], ALU.mult, ALU.mult,
                ["sg", "oml", "enb"], ["kdT"])
        tt("pool", qdT.rearrange("p h t -> p (h t)"), qs[:, :], logf[:, :], ALU.mult, ["qs", "logf"], ["qdT"])
        tt("pool", keT.rearrange("p h (c t) -> p (h c) t", t=32), kdT.rearrange("p h (c t) -> p (h c) t", t=32),
           dec[:, :].unsqueeze(2).to_broadcast([128, 16, 32]), ALU.mult, ["kdT", "dec"], ["keT"])
        tt("pool", gsn[:, :], gsn[:, :], HNb[:, :], ALU.mult, ["gsn", "HNb"], ["gsn"])
        pkt, kkt = psnew("HG", "ket")
        pktv = pkt[:, :].bitcast(BF16)
        for h in range(4):
            P.add("pe", lambda e, h=h: e.transpose(out=pktv[:, h * 128:(h + 1) * 128], in_=keT[:, h, :], identity=ident),
                  reads=["keT", "cb"], writes=[kkt], cost=0.09)
        cp("act", ke_tm.rearrange("p h k -> p (h k)"), pktv[:, 0:512], [kkt], ["ke_tm"])
        pat, kat = psnew("HG", "aT")
        for h in range(4):
            mm(pat[:, h * 128:(h + 1) * 128], kdT[:, h, :], qdT[:, h, :], True, True, ["kdT", "qdT"], [kat])
        tt("dve", aTm[:, :, :], pat[:, :].rearrange("p (h c) -> p h c", c=128), CM.unsqueeze(1).to_broadcast([128, 4, 128]),
           ALU.mult, [kat, "cb"], ["aTm"])
        for h in range(4):
            pv_, pk = psnew("HG", "U")
            mm(pv_[:, :].rearrange("p (j v) -> p j v", v=128), ke_tm[:, h, :], Vz[:, :, h * 128:(h + 1) * 128], True, True,
               ["ke_tm", "Vz"], [pk])
            for j in range(4):
                sp_ap = Sprev[:, h, :] if j == 0 else Sb[:, h, j - 1, :]
                sp_key = "Sprev" if j == 0 else ("Sb", h, j - 1)
                stt(Sb[:, h, j, :], sp_ap, dec[:, 4 * h + j:4 * h + j + 1], pv_[:, j * 128:(j + 1) * 128], ALU.mult, ALU.add,
                    [sp_key, "dec", pk], [("Sb", h, j)])
        pho, kho = psnew("HG", "o")
        for h in range(4):
            for j in range(4):
                sp_ap = Sprev[:, h, :] if j == 0 else Sb[:, h, j - 1, :]
                sp_key = "Sprev" if j == 0 else ("Sb", h, j - 1)
                o_ap = pho[32 * j:32 * j + 32, h * 128:(h + 1) * 128]
                mm(o_ap, aTm[:, h, 32 * j:32 * j + 32], v_bf[:, h * 128:(h + 1) * 128], True, False, ["aTm", "v_bf"], [kho],
                   tp=(0, 32 * j))
                mm(o_ap, qdT[:, h, 32 * j:32 * j + 32], sp_ap, False, True, ["qdT", sp_key], [kho], tp=(0, 32 * j))
        P.add("pool", lambda e: e.tensor_copy(out=Sprev[:, :, :], in_=Sb[:, :, 3, :]),
              reads=[("Sb", h, 3) for h in range(4)], writes=["Sprev"])
        cp("act", osb[:, :], pho[:, :], [kho], ["osb"])
        act(th[:, :], osb[:, :], AF.Square, ["osb"], ["th"])
        P.add("dve", lambda e: e.tensor_reduce(out=smA[:, 20:24], in_=th[:, :].rearrange("p (h v) -> p h v", v=128), axis=AX.X, op=ALU.add),
              reads=["th"], writes=["ssh"])
        rstd(smA[:, 20:24], 1.0 / 128, smA[:, 28:32], smA[:, 24:28], "ssh", "lnh", "rsh")
        tt("dve", sg[:, :].rearrange("p (h v) -> p h v", v=128), osb[:, :].rearrange("p (h v) -> p h v", v=128),
           smA[:, 28:32].unsqueeze(2).to_broadcast([128, 4, 128]), ALU.mult, ["osb", "rsh"], ["sg"])
        tt("pool", CATb[:, 512:1024], sg[:, :], gsn[:, :], ALU.mult, ["sg", "gsn"], ["CATb"])
        if debug:
            dma("sp", cat_d.ap()[rows, :], CATb[:, :], ["CATb"], [("cat_d", i)])
        transpose_tm(CATb, "CATb", catT, "catT", "LT")
        pmx = []
        for n in range(2):
            pm_, km_ = psnew("LT", "mix")
            pmx.append((pm_, km_))
            for k in range(8):
                mm(pm_[:, :], catT[:, k, :], w_out_bf[:, k, n * 512:(n + 1) * 512], k == 0, k == 7,
                   ["catT", "w_out_bf"], [km_])
            act(junk[:, n * 512:(n + 1) * 512], pm_[:, :], AF.Square, [km_], ["ssm%d" % n], accum=smA[:, 40 + n:41 + n])
        tt("dve", smA[:, 32:33], smA[:, 40:41], smA[:, 41:42], ALU.add, ["ssm0", "ssm1"], ["ssm"])
        rstd(smA[:, 32:33], 1.0 / 1024, smA[:, 34:35], smA[:, 33:34], "ssm", "lnm", "rsm")
        for n in range(2):
            pm_, km_ = pmx[n]
            stt(tA2[:, n * 512:(n + 1) * 512], pm_[:, :], smA[:, 34:35], G1b[:, n * 512:(n + 1) * 512], ALU.mult, ALU.mult,
                [km_, "rsm", "G1b"], ["tA2"])
        tt("pool", xt[:, :], tA2[:, :], xt[:, :], ALU.add, ["tA2", xk], [xk])
        dma("sp", x1_d.ap()[rows, :], xt[:, :], [xk], [("x1_d", i)])
        act(junk[:, :], xt[:, :], AF.Square, [xk], ["ss2"], accum=smA[:, 36:37])
        rstd(smA[:, 36:37], 1.0 / 1024, smA[:, 38:39], smA[:, 37:38], "ss2", "ln2", "rs2")
        stt(tA3[:, :], xt[:, :], smA[:, 38:39], GS2b[:, :], ALU.mult, ALU.mult, [xk, "rs2", "GS2b"], ["tA3"])
        tt("pool", H2b[:, :], tA3[:, :], SH2b[:, :], ALU.add, ["tA3", "SH2b"], ["H2b"])
        dma("sp", h2_d.ap()[rows, :], H2b[:, :], ["H2b"], [("h2_d", i)])
        transpose_tm(H2b, "H2b", h2T, "h2T", "LT")
        psR, kpr = psnew("LT", "lg")
        for k in range(8):
            mm(psR[:, 0:36], h2T[:, k, :], w_r_bf[:, k, :], k == 0, k == 7, ["h2T", "w_r_bf"], [kpr])
        R_ = smR
        g8, le = R_[:, 0:8], R_[:, 8:40]
        gm, gif, ngm, gsum, gw = R_[:, 40:48], R_[:, 48:49], R_[:, 49:50], R_[:, 50:51], R_[:, 51:52]
        ohg, esel, em, eif = R_[:, 52:56], R_[:, 56:64], R_[:, 64:72], R_[:, 72:74]
        dd, ex, den2, w0 = R_[:, 74:75], R_[:, 75:76], R_[:, 76:77], R_[:, 77:78]
        t48, r01, sif, gex = R_[:, 80:112], R_[:, 112:144], R_[:, 144:146], R_[:, 148:152]
        cp("dve", g8[:, 4:8], cfc("NEG", 4), ["cf"], ["g8"])
        tt("dve", g8[:, 0:4], psR[:, 0:4], BRb[:, 0:4], ALU.add, [kpr, "BRb"], ["g8"])
        tt("dve", le, psR[:, 4:36], BRb[:, 4:36], ALU.add, [kpr, "BRb"], ["le"])
        P.add("dve", lambda e: e.max(out=gm, in_=g8), reads=["g8"], writes=["gm"])
        P.add("dve", lambda e: e.max_index(out=smU[:, 0:8], in_max=gm, in_values=g8), reads=["g8", "gm"], writes=["gi"])
        cp("dve", gif, smU[:, 0:1], ["gi"], ["gif"])
        ts("dve", ngm, gm[:, 0:1], -1.0, ALU.mult, ["gm"], ["ngm"])
        act(gex, g8[:, 0:4], AF.Exp, ["g8", "ngm"], ["gex", "gsum"], bias=ngm, accum=gsum)
        P.add("dve", lambda e: e.reciprocal(out=gw, in_=gsum), reads=["gsum"], writes=["gw"])
        ts("dve", ohg, cfc("IO32", 4), gif, ALU.is_equal, ["cf", "gif"], ["ohg"])
        tt("dve", t48.rearrange("p (g e) -> p g e", e=8), le.rearrange("p (g e) -> p g e", e=8),
           ohg.unsqueeze(2).to_broadcast([128, 4, 8]), ALU.mult, ["le", "ohg"], ["t48"])
        P.add("dve", lambda e: e.tensor_reduce(out=esel, in_=t48.rearrange("p (g e) -> p e g", e=8), axis=AX.X, op=ALU.add),
              reads=["t48"], writes=["esel"])
        P.add("dve", lambda e: e.max(out=em, in_=esel), reads=["esel"], writes=["em"])
        P.add("dve", lambda e: e.max_index(out=smU[:, 8:16], in_max=em, in_values=esel), reads=["esel", "em"], writes=["ei"])
        cp("dve", eif, smU[:, 8:10], ["ei"], ["eif"])
        tt("dve", dd, em[:, 1:2], em[:, 0:1], ALU.subtract, ["em"], ["dd"])
        act(ex, dd, AF.Exp, ["dd"], ["ex"])
        ts("dve", den2, ex, 1.0, ALU.add, ["ex"], ["den2"])
        P.add("dve", lambda e: e.reciprocal(out=w0, in_=den2), reads=["den2"], writes=["w0"])
        tt("dve", WT[:, 2 * i:2 * i + 1], w0, gw, ALU.mult, ["w0", "gw"], ["WT"])
        stt(WT[:, 2 * i + 1:2 * i + 2], ex, w0, gw, ALU.mult, ALU.mult, ["ex", "w0", "gw", "WT"], ["WT"])
        EG2 = EG[:, 2 * i:2 * i + 2]
        RK2 = RK[:, 2 * i:2 * i + 2]
        stt(EG2, gif.to_broadcast([128, 2]), 8.0, eif, ALU.mult, ALU.add, ["gif", "eif"], ["EG"])
        for k_ in range(2):
            ts("dve", O01[:, 32 * k_:32 * k_ + 32], cfc("IO32", 32), EG[:, 2 * i + k_:2 * i + k_ + 1], ALU.is_equal, ["cf", "EG"], ["O%d" % k_])
        O0, O1 = O01[:, 0:32], O01[:, 32:64]
        psR, kpr = psnew("LT", "rk")
        mm(psR[:, 64:96], Lst, O0, True, True, ["cb", "O0"], [kpr])
        mm(psR[:, 96:128], Lst, O1, True, False, ["cb", "O1"], [kpr])
        mm(psR[:, 96:128], ones_bf, O0, False, True, ["cb", "O0"], [kpr])
        mm(psR[:, 128:160], ones_bf, O0, True, False, ["cb", "O0"], [kpr])
        mm(psR[:, 128:160], ones_bf, O1, False, True, ["cb", "O1"], [kpr])
        for k_ in range(2):
            tt("dve", r01, psR[:, 64 + 32 * k_:96 + 32 * k_], Cb[:, :], ALU.add, [kpr, "Cb"], ["r01"])
            stt(t48, r01, 1.0, O01[:, 32 * k_:32 * k_ + 32], ALU.mult, ALU.mult, ["r01", "O%d" % k_], ["t48", "RK"],
                accum=RK[:, 2 * i + k_:2 * i + k_ + 1])
        tt("dve", Cb[:, :], Cb[:, :], psR[:, 128:160], ALU.add, ["Cb", kpr], ["Cb"])
        stt(sif, EG2, float(CAP), RK2, ALU.mult, ALU.add, ["EG", "RK"], ["sif"])
        cp("dve", smI[:, 0:2], sif, ["sif"], ["smI"])
        for k_ in range(2):
            P.add("pool", lambda e, k_=k_: e.indirect_dma_start(out=inv_d.ap(), out_offset=bass.IndirectOffsetOnAxis(ap=smI[:, k_:k_ + 1], axis=0),
                                                                in_=ci[:, i:i + 1], in_offset=None),
                  reads=["smI", "ci"] + INV0_KEYS, writes=[("inv", i, k_)], dma=True, cost=1.0, dmat=0.05)

    njob = 0
    if _os.environ.get("NOCONV") == "1":
        jobs = []
    for i in range(NT):
        tileA(i)
        CONV_GATE[0] = [("x1_d", i)]
        tgt = (len(jobs) * (i + 1) + NT - 1) // NT
        while njob < min(tgt, len(jobs)):
            conv_job(jobs[njob])
            njob += 1
    while njob < len(jobs):
        conv_job(jobs[njob])
        njob += 1

    P.barrier([
        ("act", lambda e: e.copy(out=dmy[:, 0:8], in_=dmy[:, 0:8])),
        ("dve", lambda e: e.memset(dmy[:, 8:16], 0.0)),
        ("pool", lambda e: e.memset(dmy[:, 16:24], 0.0)),
    ])

    NTM = NT
    per = min(32, 1024 // NTM)
    for e0 in range(0, 32, per):
        bv = big[:, 0:per * NTM].rearrange("p (e m) -> p e m", m=NTM)
        tt("dve", bv, Cb[:, e0:e0 + per].unsqueeze(2).to_broadcast([128, per, NTM]),
           cfc("MG", NTM).unsqueeze(1).to_broadcast([128, per, NTM]), ALU.is_gt, ["Cb", "cf"], ["big"])
        P.add("dve", lambda e, e0=e0, bv=bv: e.tensor_reduce(out=ntl[:, e0:e0 + per], in_=bv, axis=AX.X, op=ALU.add),
              reads=["big"], writes=["ntl"])
    P.add("dve", lambda e: e.memset(big[:, 0:32], 1.0), reads=["ntl"], writes=["big"])
    P.add("dve", lambda e: e.tensor_tensor_scan(out=tend[:, :], data0=big[:, 0:32], data1=ntl[:, :], initial=0.0, op0=ALU.mult, op1=ALU.add),
          reads=["big", "ntl"], writes=["tend"])
    tt("dve", tst[:, :], tend[:, :], ntl[:, :], ALU.subtract, ["tend", "ntl"], ["tst"])
    ts("dve", pst[:, :], tst[:, :], 128.0, ALU.mult, ["tst"], ["pst"])
    for c0 in range(0, NSLT, 32):
        n_ = min(32, NSLT - c0)
        bv = big[:, 0:n_ * 32].rearrange("p (c e) -> p c e", e=32)
        tt("dve", bv, tend[:, :].unsqueeze(1).to_broadcast([128, n_, 32]), cfc("JG", n_, c0).unsqueeze(2).to_broadcast([128, n_, 32]),
           ALU.is_le, ["tend", "cf"], ["big"])
        P.add("dve", lambda e, c0=c0, n_=n_, bv=bv: e.tensor_reduce(out=EXJ[:, c0:c0 + n_], in_=bv, axis=AX.X, op=ALU.add),
              reads=["big"], writes=["EXJ"])
    ts("dve", EXJ[:, :], EXJ[:, :], 31.0, ALU.min, ["EXJ"], ["EXJ"])
    P.add("dve", lambda e: e.memset(TSJ[:, 0:1], 0.0), reads=[], writes=["TSJ"])
    tt("dve", TSJ[:, 1:NSLT], EXJ[:, 1:NSLT], EXJ[:, 0:NSLT - 1], ALU.is_equal, ["EXJ"], ["TSJ"])
    P.add("dve", lambda e: e.tensor_tensor_scan(out=RJ[:, :], data0=TSJ[:, :], data1=TSJ[:, :], initial=0.0, op0=ALU.mult, op1=ALU.add),
          reads=["TSJ"], writes=["RJ"], cost=0.5)
    ts("dve", IJF[:, :], RJ[:, :], 128.0, ALU.mult, ["RJ"], ["IJF"], s2=cfc("PIDX"), op1=ALU.add)
    stt(IJF[:, :], EXJ[:, :], float(CAP), IJF[:, :], ALU.mult, ALU.add, ["EXJ", "IJF"], ["IJF"])
    ts("dve", IJF[:, :], IJF[:, :], float(NINV - 1), ALU.min, ["IJF"], ["IJF"])
    cp("dve", IDXJ[:, :], IJF[:, :], ["IJF"], ["IDXJ"])
    ts("dve", IJF[:, :], EXJ[:, :], 128.0, ALU.mult, ["EXJ", "IDXJ"], ["IJF"], s2=cfc("PIDX"), op1=ALU.add)
    for r_ in range(1, NWB):
        if r_ * SST < NSLT:
            P.add("dve", lambda e, r_=r_: e.memset(TSJ[:, r_ * SST:r_ * SST + 1], 0.0), reads=["RJ"], writes=["TSJ"])
    stt(IJF[:, 1:NSLT], TSJ[:, 1:NSLT], 1.0e6, IJF[:, 1:NSLT], ALU.mult, ALU.add, ["TSJ", "IJF"], ["IJF"])
    cp("dve", WIDX[:, :], IJF[:, :], ["IJF"], ["WIDX"])

    def fetchT(j, r6):
        P.add("pool", lambda e: e.indirect_dma_start(out=tokidx[r6][:, 0:1], out_offset=None, in_=inv_d.ap(),
                                                     in_offset=bass.IndirectOffsetOnAxis(ap=IDXJ[:, j:j + 1], axis=0)),
              reads=["IDXJ"] + INV_KEYS + INV0_KEYS, writes=["tokidx%d" % r6], dma=True, cost=1.0, dmat=0.05)

    def fetchX(j, r3, r6):
        P.add("pool", lambda e: e.indirect_dma_start(out=xg[r3][:, :], out_offset=None, in_=h2_d.ap(),
                                                     in_offset=bass.IndirectOffsetOnAxis(ap=tokidx[r6][:, 0:1], axis=0)),
              reads=["tokidx%d" % r6] + H2_KEYS, writes=["xg%d" % r3], dma=True, cost=1.0, dmat=1.05)

    def fetchW(j, rw):
        P.add("pool", lambda e: e.indirect_dma_start(out=wall[rw][:, :], out_offset=None, in_=wall_d.ap(),
                                                     in_offset=bass.IndirectOffsetOnAxis(ap=WIDX[:, j:j + 1], axis=0),
                                                     bounds_check=BC_REG[0], oob_is_err=False),
              reads=["WIDX"] + WGU_KEYS + WDN_KEYS, writes=["wall%d" % rw], dma=True, cost=1.0, dmat=3.0)

    def tileB(j, rw, r3, r2):
        gT = xgT[r2]
        ptb, ktb = psnew("ALL", "trB")
        ptbv = ptb[:, :].bitcast(BF16)
        for c in range(8):
            P.add("pe", lambda e, c=c: e.transpose(out=ptbv[:, c * 128:(c + 1) * 128], in_=xg[r3][:, c * 128:(c + 1) * 128], identity=ident),
                  reads=["xg%d" % r3, "cb"], writes=[ktb], cost=0.09)
        cp("act", gT.rearrange("p k t -> p (k t)"), ptbv[:, :], [ktb], ["xgT%d" % r2])
        wv = wall[rw][:, 0:4096].rearrange("p (k n) -> p k n", n=512)
        ps_gu, kgu = psnew("ALL", "gu")
        for q_ in range(4):
            for k in range(8):
                mm(ps_gu[:, q_ * 128:(q_ + 1) * 128], wv[:, k, q_ * 128:(q_ + 1) * 128], gT[:, k, :], k == 0, k == 7,
                   ["wall%d" % rw, "xgT%d" % r2], [kgu])
        act(sgm[:, :], ps_gu[:, 0:256], AF.Silu, [kgu], ["sgm"])
        tt("dve", actT[r2].rearrange("p f t -> p (f t)"), sgm[:, :], ps_gu[:, 256:512], ALU.mult, ["sgm", kgu], ["actT%d" % r2])
        dv = wall[rw][:, 4096:6144].rearrange("p (f n) -> p f n", n=1024)
        for n in range(2):
            pd_, kd_ = psnew("ALL", "dn")
            for f in range(2):
                mm(pd_[:, :], actT[r2][:, f, :], dv[:, f, n * 512:(n + 1) * 512], f == 0, f == 1,
                   ["actT%d" % r2, "wall%d" % rw], [kd_])
            cp("act" if n == 0 else "dve", ysb[r2][:, n * 512:(n + 1) * 512], pd_[:, :], [kd_], ["ysb%d" % r2])
        dma("sp", ys_d.ap()[j * 128:(j + 1) * 128, :], ysb[r2][:, :], ["ysb%d" % r2], [("ys_d", j)])

    order = []
    for t_ in range(NWB * SST):
        j_ = (t_ % NWB) * SST + t_ // NWB
        if j_ < NSLT:
            order.append((j_, t_ % NWB))
    NB_ = len(order)
    for n_ in range(min(4, NB_)):
        fetchT(order[n_][0], n_ % 6)
    for n_ in range(min(2, NB_)):
        fetchX(order[n_][0], n_ % 3, n_ % 6)
    prev_user = {}
    lastpos = {}
    for n_ in range(NB_):
        prev_user[n_] = lastpos.get(order[n_][1])
        lastpos[order[n_][1]] = n_
    next_w = 0
    for n_ in range(NB_):
        if n_ + 4 < NB_:
            fetchT(order[n_ + 4][0], (n_ + 4) % 6)
        if n_ + 2 < NB_:
            fetchX(order[n_ + 2][0], (n_ + 2) % 3, (n_ + 2) % 6)
        while next_w < NB_ and next_w <= n_ + NWB - 1 and (prev_user[next_w] is None or prev_user[next_w] < n_):
            fetchW(order[next_w][0], order[next_w][1])
            next_w += 1
        assert next_w > n_
        tileB(order[n_][0], order[n_][1], n_ % 3, n_ % 2)

    io32b = cfc("IO32", 32).unsqueeze(1).to_broadcast([128, 32, 32])
    for c0 in range(0, 2 * NT, 32):
        n_ = min(32, 2 * NT - c0)
        bv = big[:, 0:n_ * 32].rearrange("p (c e) -> p c e", e=32)
        tt("dve", bv, cfc("IO32", 32).unsqueeze(1).to_broadcast([128, n_, 32]), EG[:, c0:c0 + n_].unsqueeze(2).to_broadcast([128, n_, 32]),
           ALU.is_equal, ["cf", "EG"], ["big"])
        tt("dve", bv, bv, pst[:, :].unsqueeze(1).to_broadcast([128, n_, 32]), ALU.mult, ["big", "pst"], ["big"])
        P.add("dve", lambda e, c0=c0, n_=n_, bv=bv: e.tensor_reduce(out=PB[:, c0:c0 + n_], in_=bv, axis=AX.X, op=ALU.add),
              reads=["big"], writes=["PB"])
    tt("dve", PB[:, :], PB[:, :], RK[:, :], ALU.add, ["PB", "RK"], ["PB"])
    cp("dve", POSI[:, :], PB[:, :], ["PB"], ["POSI"])

    def fetchC(i):
        r2 = i % 3
        P.add("pool", lambda e: e.indirect_dma_start(out=y0[r2][:, :], out_offset=None, in_=ys_d.ap(),
                                                     in_offset=bass.IndirectOffsetOnAxis(ap=POSI[:, 2 * i:2 * i + 1], axis=0)),
              reads=["POSI"] + YS_KEYS, writes=["y0_%d" % r2], dma=True, cost=1.0, dmat=1.05)
        P.add("pool", lambda e: e.indirect_dma_start(out=y1[r2][:, :], out_offset=None, in_=ys_d.ap(),
                                                     in_offset=bass.IndirectOffsetOnAxis(ap=POSI[:, 2 * i + 1:2 * i + 2], axis=0)),
              reads=["POSI"] + YS_KEYS, writes=["y1_%d" % r2], dma=True, cost=1.0, dmat=1.05)
        dma("sp", x1c[r2][:, :], x1_d.ap()[i * 128:(i + 1) * 128, :], [("x1_d", i)], ["x1c%d" % r2])

    def tileC(i):
        r2 = i % 3
        act(yc[:, :], y0[r2][:, :], AF.Identity, ["y0_%d" % r2, "WT"], ["yc"], scale=WT[:, 2 * i:2 * i + 1])
        stt(yc[:, :], y1[r2][:, :], WT[:, 2 * i + 1:2 * i + 2], yc[:, :], ALU.mult, ALU.add, ["y1_%d" % r2, "WT", "yc"], ["yc"])
        act(junkC[:, :], yc[:, :], AF.Square, ["yc"], ["junkC", "ssy"], accum=smC[:, 0:1])
        rstd(smC[:, 0:1], 1.0 / 1024, smC[:, 2:3], smC[:, 1:2], "ssy", "lny", "rsy")
        stt(tC[:, :], yc[:, :], smC[:, 2:3], G2b[:, :], ALU.mult, ALU.mult, ["yc", "rsy", "G2b"], ["tC"])
        tt("pool", x1c[r2][:, 0:512], tC[:, 0:512], x1c[r2][:, 0:512], ALU.add, ["tC", "x1c%d" % r2], [("x1o", r2, 0)])
        tt("dve", x1c[r2][:, 512:1024], tC[:, 512:1024], x1c[r2][:, 512:1024], ALU.add, ["tC", "x1c%d" % r2], [("x1o", r2, 1)])
        dma("sp", out_d.ap()[i * 128:(i + 1) * 128, :], x1c[r2][:, :], [("x1o", r2, 0), ("x1o", r2, 1)], [("out_d", i), "x1c%d" % r2])

    fetchC(0)
    if NT > 1:
        fetchC(1)
    for i in range(NT):
        if i + 2 < NT:
            fetchC(i + 2)
        tileC(i)
    P.add("sp", None, reads=[("out_d", i) for i in range(NT)] + [("x1_d", i) for i in range(NT)] + H2_KEYS + MOD_KEYS + ([("cat_d", i) for i in range(NT)] if debug else []))
    if SCHED:
        P.schedule()
    if _os.environ.get("NO_EMIT") == "1":
        return None
    stats = P.emit(nc, st)
    print("ops per engine, waits:", stats)
    st.close()
    return nc


def permute_w_in(w_in):
    q = w_in[:, 0:512].reshape(1024, 2, 4, 64).transpose(0, 2, 1, 3).reshape(1024, 512)
    return np.ascontiguousarray(np.concatenate([q, w_in[:, 512:]], axis=1))


def make_in_maps(NT, nb, inputs):
    f32 = np.float32
    cf, cb, ci = make_consts(NT)
    T = NT * 128
    shared = {
        "w_ada": np.ascontiguousarray(inputs["w_ada"][0], f32),
        "b_ada": np.ascontiguousarray(inputs["b_ada"][0:1], f32),
        "w_in": permute_w_in(np.asarray(inputs["w_in"][0], f32)),
        "w_out": np.ascontiguousarray(inputs["w_out"][0], f32),
        "w_r": np.ascontiguousarray(np.concatenate([inputs["w_router_group"][0], inputs["w_router_expert"][0]], axis=1), f32),
        "b_r": np.ascontiguousarray(np.concatenate([inputs["b_router_group"][0], inputs["b_router_expert"][0]])[None, :], f32),
        "ln1_pre": np.ascontiguousarray(inputs["ln1_pre"][0:1], f32),
        "ln1_post": np.ascontiguousarray(inputs["ln1_post"][0:1], f32),
        "ln2_pre": np.ascontiguousarray(inputs["ln2_pre"][0:1], f32),
        "ln2_post": np.ascontiguousarray(inputs["ln2_post"][0:1], f32),
        "sinks": np.ascontiguousarray(inputs["attn_sinks"][0:1], f32),
        "attn_norm": np.ascontiguousarray(inputs["attn_out_norm"][0:1], f32),
        "hgrn_norm": np.ascontiguousarray(inputs["hgrn_out_norm"][0:1], f32),
        "lbc": np.ascontiguousarray(np.asarray(inputs["hgrn_lb"], f32).reshape(2, 4, 128).transpose(2, 0, 1).reshape(128, 8)),
        "w_gate": np.ascontiguousarray(inputs["w_exp_gate"][0], f32),
        "w_up": np.ascontiguousarray(inputs["w_exp_up"][0], f32),
        "w_down": np.ascontiguousarray(inputs["w_exp_down"][0], f32),
        "cf": cf, "cb": cb, "ci": ci,
    }
    maps = []
    for b in range(nb):
        m = dict(shared)
        m["x"] = np.ascontiguousarray(inputs["x"][b, :T], f32)
        m["ccol"] = np.ascontiguousarray(np.asarray(inputs["c"][b], f32).reshape(8, 128).T)
        maps.append(m)
    return maps


_NC_CACHE = {}


def kernel(**inputs):
    NT = 64
    if NT not in _NC_CACHE:
        _NC_CACHE[NT] = build(NT)
    nc = _NC_CACHE[NT]
    maps = make_in_maps(NT, 8, inputs)
    res = run_bass_kernel_spmd(nc, maps, core_ids=list(range(8)))
    out = np.stack([np.asarray(r["out"], np.float32) for r in res.results], axis=0)
    return out
```
